# Optimizing a Trainium2 kernel written in Bass

```python
import jax, jax.numpy as jnp
from jax import lax
import numpy as np

D_MODEL = 1024
BATCH = 32
SEQ = 2048
DEPTH = 1

CTX_LEN = 256
GRID_W = 64
D_F = D_MODEL // 2
N_FG = 4
FG = D_F // N_FG
D_M = D_MODEL // 2
N_H = 4
DH = D_M // N_H
CHUNK = 64
CONV_K = 3
N_GATE_KINDS = 4
FORGET_BIAS_LO = 3.0
FORGET_BIAS_HI = 6.0
N_BRANCH = 2
N_EXPERTS = 16
CAP_FACTOR = 2
D_FF_E = D_MODEL
EPS = 1e-6
OFF_F = 0
OFF_QK = OFF_F + D_F
OFF_V = OFF_QK + 2 * D_M
OFF_O = OFF_V + D_M
OFF_G = OFF_O + D_M
OFF_BR = OFF_G + N_GATE_KINDS * N_H
IN_COLS = OFF_BR + N_BRANCH * D_MODEL

kernel_name = 'hybrid_fourier_mlstm_ec_block'


def rmsnorm(x, g):
    xf = x.astype(jnp.float32)
    y = xf * lax.rsqrt(jnp.mean(xf * xf, axis=-1, keepdims=True) + EPS)
    return (y * g.astype(jnp.float32)).astype(x.dtype)


def modulate(h, shift, scale):
    return h * (1 + scale) + shift


def grid_conv(u, w, rows):
    b, l, ch = u.shape
    ug = u.reshape(b, rows, l // rows, ch)
    out = lax.conv_general_dilated(ug, w[:, :, None, :].astype(u.dtype), (1, 1), 'SAME',
                                   dimension_numbers=('NHWC', 'HWIO', 'NHWC'),
                                   feature_group_count=ch)
    return out.reshape(b, l, ch)


def mlstm_inputs(p, rows, w_conv, b_gates):
    b, l, _ = p.shape
    qk = jax.nn.silu(grid_conv(p[..., OFF_QK:OFF_V], w_conv, rows))
    to_heads = lambda a: a.reshape(b, l, N_H, DH).transpose(0, 2, 1, 3).astype(jnp.float32)
    q = to_heads(qk[..., :D_M])
    k = to_heads(qk[..., D_M:]) * (DH ** -0.5)
    v = to_heads(p[..., OFF_V:OFF_O])
    g = (p[..., OFF_G:OFF_BR].astype(jnp.float32).reshape(b, l, N_GATE_KINDS, N_H).transpose(2, 0, 3, 1)
         + b_gates.astype(jnp.float32)[:, None, :, None])
    return q, k, v, g


def zero_state(b):
    return (jnp.zeros((b, N_H, DH, DH), jnp.float32), jnp.zeros((b, N_H, DH), jnp.float32),
            jnp.zeros((b, N_H), jnp.float32))


def mlstm_scan(q, k, v, ig, fl, state):
    b, h, l, dh = q.shape
    nc = l // CHUNK

    def chunks(a):
        return jnp.moveaxis(a.reshape((b, h, nc, CHUNK) + a.shape[3:]), 2, 0)

    tril = jnp.tril(jnp.ones((CHUNK, CHUNK), dtype=bool))

    def step(carry, inp):
        c0, n0, m0 = carry
        qc, kc, vc, ic, fc = inp
        cum = jnp.cumsum(fc, axis=-1)
        logw = jnp.where(tril, cum[..., :, None] - cum[..., None, :] + ic[..., None, :], -jnp.inf)
        inter = cum + m0[..., None]
        m = jnp.maximum(inter, logw.max(-1))
        s = jnp.einsum('bhjd,bhsd->bhjs', qc, kc) * jnp.exp(logw - m[..., None])
        dec = jnp.exp(inter - m)
        num = jnp.einsum('bhjs,bhsd->bhjd', s, vc) + dec[..., None] * jnp.einsum('bhjk,bhkv->bhjv', qc, c0)
        den = s.sum(-1) + dec * jnp.einsum('bhjk,bhk->bhj', qc, n0)
        hc = num / jnp.maximum(jnp.abs(den), jnp.exp(-m))[..., None]
        tot = cum[..., -1]
        a = tot[..., None] - cum + ic
        m_new = jnp.maximum(tot + m0, a.max(-1))
        wa = jnp.exp(a - m_new[..., None])
        dn = jnp.exp(tot + m0 - m_new)
        c_new = dn[..., None, None] * c0 + jnp.einsum('bhs,bhsk,bhsv->bhkv', wa, kc, vc)
        n_new = dn[..., None] * n0 + jnp.einsum('bhs,bhsk->bhk', wa, kc)
        return (c_new, n_new, m_new), hc

    final, hs = lax.scan(step, state, (chunks(q), chunks(k), chunks(v), chunks(ig), chunks(fl)))
    return jnp.moveaxis(hs, 0, 2).reshape(b, h, l, dh), final


def bidir_mlstm(q, k, v, g, init):
    flip = lambda a: jnp.flip(a, axis=2)
    h_f, s_f = mlstm_scan(q, k, v, g[0], jax.nn.log_sigmoid(g[1]), init[0])
    h_b, s_b = mlstm_scan(flip(q), flip(k), flip(v), flip(g[2]), flip(jax.nn.log_sigmoid(g[3])), init[1])
    return h_f + flip(h_b), (s_f, s_b)


def branch_merge(p, h_m, w_fourier, w_mlstm, w_out):
    b, l, _ = p.shape
    u = p[..., OFF_F:OFF_QK].astype(jnp.float32).reshape(b, l, N_FG, FG)
    fu = jnp.fft.fft2(u, axes=(1, 3), norm='ortho').real.astype(p.dtype).reshape(b, l, D_F)
    y_f = fu @ w_fourier
    hm = h_m.transpose(0, 2, 1, 3).reshape(b, l, D_M).astype(p.dtype)
    y_m = (jax.nn.sigmoid(p[..., OFF_O:OFF_G]) * hm) @ w_mlstm
    gates = jax.nn.sigmoid(p[..., OFF_BR:])
    merged = gates[..., :D_MODEL] * y_f + gates[..., D_MODEL:] * y_m
    return merged @ w_out


def expert_choice(h, w_router, w_gate, w_up, w_down):
    b, l, _ = h.shape
    cap = CAP_FACTOR * l // N_EXPERTS
    aff = jax.nn.softmax(jnp.einsum('bld,de->ble', h, w_router).astype(jnp.float32), axis=-1)
    vals, idx = lax.top_k(jnp.swapaxes(aff, 1, 2), cap)
    bidx = jnp.arange(b)[:, None, None]
    xs = h[bidx, idx]
    hid = jax.nn.silu(jnp.einsum('becd,edf->becf', xs, w_gate)) * jnp.einsum('becd,edf->becf', xs, w_up)
    y = jnp.einsum('becf,efd->becd', hid, w_down) * vals[..., None].astype(h.dtype)
    return jnp.zeros_like(h).at[bidx, idx].add(y)


def setup_inputs(seed: int = 0) -> dict:
    key = jax.random.key(seed)
    ks = jax.random.split(key, 20)
    nrm = jax.random.normal
    forget = jnp.linspace(FORGET_BIAS_LO, FORGET_BIAS_HI, N_H)
    zeros_h = jnp.zeros((N_H,))
    gate_base = jnp.stack([zeros_h, forget, zeros_h, forget])
    return {
        'x': nrm(ks[0], (BATCH, SEQ, D_MODEL), jnp.float32),
        'c': nrm(ks[1], (BATCH, D_MODEL), jnp.float32),
        'ctx': nrm(ks[2], (BATCH, CTX_LEN, D_MODEL), jnp.float32),
        'c_ctx': nrm(ks[3], (D_MODEL,), jnp.float32),
        'w_ada': nrm(ks[4], (DEPTH, D_MODEL, 6 * D_MODEL), jnp.float32) * (0.5 * D_MODEL ** -0.5),
        'b_ada': nrm(ks[5], (DEPTH, 6 * D_MODEL), jnp.float32) * 0.02,
        'g_norm1': 1.0 + 0.02 * nrm(ks[6], (DEPTH, D_MODEL), jnp.float32),
        'w_in': nrm(ks[7], (DEPTH, D_MODEL, IN_COLS), jnp.float32) * D_MODEL ** -0.5,
        'b_gates': gate_base[None] + 0.1 * nrm(ks[8], (DEPTH, N_GATE_KINDS, N_H), jnp.float32),
        'w_conv': nrm(ks[9], (DEPTH, CONV_K, CONV_K, 2 * D_M), jnp.float32) * (1.0 / CONV_K),
        'w_fourier': nrm(ks[10], (DEPTH, D_F, D_MODEL), jnp.float32) * D_F ** -0.5,
        'w_mlstm': nrm(ks[11], (DEPTH, D_M, D_MODEL), jnp.float32) * D_M ** -0.5,
        'w_out': nrm(ks[12], (DEPTH, D_MODEL, D_MODEL), jnp.float32) * D_MODEL ** -0.5,
        'g_norm2': 1.0 + 0.02 * nrm(ks[13], (DEPTH, D_MODEL), jnp.float32),
        'w_router': nrm(ks[14], (DEPTH, D_MODEL, N_EXPERTS), jnp.float32) * D_MODEL ** -0.5,
        'w_gate_e': nrm(ks[15], (DEPTH, N_EXPERTS, D_MODEL, D_FF_E), jnp.float32) * D_MODEL ** -0.5,
        'w_up_e': nrm(ks[16], (DEPTH, N_EXPERTS, D_MODEL, D_FF_E), jnp.float32) * D_MODEL ** -0.5,
        'w_down_e': nrm(ks[17], (DEPTH, N_EXPERTS, D_FF_E, D_MODEL), jnp.float32) * D_FF_E ** -0.5,
        'g_final': 1.0 + 0.02 * nrm(ks[18], (D_MODEL,), jnp.float32),
    }


def reference(x, c, ctx, c_ctx, w_ada, b_ada, g_norm1, w_in, b_gates, w_conv, w_fourier, w_mlstm,
              w_out, g_norm2, w_router, w_gate_e, w_up_e, w_down_e, g_final):
    b, seq, _ = x.shape
    rows = seq // GRID_W
    for l in range(DEPTH):
        last = l == DEPTH - 1
        mx = (jax.nn.silu(c) @ w_ada[l] + b_ada[l])[:, None, :]
        sh1, sc1, ga1, sh2, sc2, ga2 = jnp.split(mx, 6, axis=-1)
        mc = jax.nn.silu(c_ctx) @ w_ada[l] + b_ada[l]
        csh1, csc1, cga1, csh2, csc2, cga2 = jnp.split(mc, 6, axis=-1)
        pc = modulate(rmsnorm(ctx, g_norm1[l]), csh1, csc1) @ w_in[l]
        qc, kc, vc, gc = mlstm_inputs(pc, 1, w_conv[l], b_gates[l])
        h_mc, ctx_states = bidir_mlstm(qc, kc, vc, gc, (zero_state(b), zero_state(b)))
        px = modulate(rmsnorm(x, g_norm1[l]), sh1, sc1) @ w_in[l]
        qx, kx, vx, gx = mlstm_inputs(px, rows, w_conv[l], b_gates[l])
        h_mx, _ = bidir_mlstm(qx, kx, vx, gx, ctx_states)
        x = x + ga1 * branch_merge(px, h_mx, w_fourier[l], w_mlstm[l], w_out[l])
        hx2 = modulate(rmsnorm(x, g_norm2[l]), sh2, sc2)
        x = x + ga2 * expert_choice(hx2, w_router[l], w_gate_e[l], w_up_e[l], w_down_e[l])
        if not last:
            ctx = ctx + cga1 * branch_merge(pc, h_mc, w_fourier[l], w_mlstm[l], w_out[l])
            hc2 = modulate(rmsnorm(ctx, g_norm2[l]), csh2, csc2)
            ctx = ctx + cga2 * expert_choice(hc2, w_router[l], w_gate_e[l], w_up_e[l], w_down_e[l])
    return rmsnorm(x, g_final)
```

```python
import numpy as np
import ml_dtypes
from contextlib import ExitStack
import concourse.bass as bass
import concourse.mybir as mybir
from concourse.bass_utils import run_bass_kernel_spmd

F32 = mybir.dt.float32
BF16 = mybir.dt.bfloat16
I32 = mybir.dt.int32
U32 = mybir.dt.uint32
AF = mybir.ActivationFunctionType
ALU = mybir.AluOpType

D = 1024
L = 2048
NT = L // 128
CL = 256
NE = 16
CAP = 256
DH = 128
OFF_F, OFF_QK, OFF_V, OFF_O, OFF_G, OFF_BR, IN_COLS = 0, 512, 1536, 2048, 2560, 2576, 4624
EPS = 1e-6
NCORES = 8
NBFULL = 4
SAME_ENGINE_NOWAIT = ("pe",)


class Prog:
    ENG = ("pe", "act", "dve", "pool", "sp")
    LIMIT = 28000

    def __init__(self, nc, es):
        self.nc = nc
        self.es = es
        self.h = {"pe": nc.tensor, "act": nc.scalar, "dve": nc.vector, "pool": nc.gpsimd, "sp": nc.sync}
        self.count = {e: 0 for e in self.ENG}
        self.epoch = {e: 0 for e in self.ENG}
        self.last_tok = {e: None for e in self.ENG}
        self.sem = {}
        self.seen = {e: {} for e in self.ENG}
        self.lastw = {}
        self.readers = {}
        self.rings = {}
        self.nsem = 0
        self.bank_last = {}
        for e in self.ENG:
            self._newsem(e, 0)
        for q, n in (("sp", 20), ("pool", 10), ("act", 6)):
            names = []
            for j in range(n):
                nm = f"{q}_d{j}"
                self.sem[(nm, 0)] = es.enter_context(nc.semaphore(nm))
                names.append(nm)
            self.rings[q] = dict(names=names, vals=[0] * n, nxt=0)

    def _newsem(self, e, ep):
        self.sem[(e, ep)] = self.es.enter_context(self.nc.semaphore(f"s_{e}_{ep}"))

    def _wait(self, e, tok):
        src, ep, val = tok
        if src == e and e in SAME_ENGINE_NOWAIT:
            return
        cur = self.seen[e].get(src, (-1, 0))
        if (ep, val) <= cur:
            return
        self.h[e].wait_ge(self.sem[(src, ep)], val)
        self.seen[e][src] = (ep, val)

    def _deps(self, e, reads, writes):
        toks = set()
        for k in reads:
            t = self.lastw.get(k)
            if t is not None:
                toks.add(t)
        for k in writes:
            t = self.lastw.get(k)
            if t is not None:
                toks.add(t)
            for t in self.readers.get(k, {}).values():
                toks.add(t)
        for t in sorted(toks):
            self._wait(e, t)

    def _record(self, tok, reads, writes):
        for k in reads:
            self.readers.setdefault(k, {})[tok[0]] = tok
        for k in writes:
            self.lastw[k] = tok
            self.readers[k] = {}

    @staticmethod
    def _bank_of(k):
        if isinstance(k, tuple) and len(k) >= 2:
            if k[0] == "psb":
                return k[1]
            if k[0] == "ps0":
                return 0
            if k[0] == "ps1":
                return 1
            if k[0] == "pO":
                return 2 + (k[1] % 2)
        return None

    def op(self, e, fn, reads=(), writes=(), inc=True):
        assert inc or e == "pe"
        self._deps(e, reads, writes)
        banks = {self._bank_of(k) for k in tuple(reads) + tuple(writes)} - {None}
        for bk in sorted(banks):
            for oe, t in sorted(self.bank_last.get(bk, {}).items()):
                if oe != e:
                    self._wait(e, t)
        ins = fn(self.h[e])
        if inc:
            self.count[e] += 1
            ins.then_inc(self.sem[(e, self.epoch[e])], 1)
            tok = (e, self.epoch[e], self.count[e])
            self.last_tok[e] = tok
            if self.count[e] >= self.LIMIT:
                self.epoch[e] += 1
                self.count[e] = 0
                self._newsem(e, self.epoch[e])
        else:
            tok = (e, self.epoch[e], self.count[e] + 1)
        for bk in banks:
            self.bank_last.setdefault(bk, {})[e] = tok
        self._record(tok, reads, writes)
        return tok

    def dma(self, q, out, in_, reads=(), writes=(), fn=None):
        ring = self.rings[q]
        j = ring["nxt"]
        ring["nxt"] = (j + 1) % len(ring["names"])
        nm = ring["names"][j]
        if ring["vals"][j] > 0:
            self._wait(q, (nm, 0, ring["vals"][j]))
        self._deps(q, reads, writes)
        if fn is None:
            ins = self.h[q].dma_start(out=out, in_=in_)
        else:
            ins = fn(self.h[q])
        ins.then_inc(self.sem[(nm, 0)], 16)
        ring["vals"][j] += 16
        tok = (nm, 0, ring["vals"][j])
        self._record(tok, reads, writes)
        return tok

    def barrier(self):
        toks = [t for t in self.last_tok.values() if t is not None]
        for ring in self.rings.values():
            for nm, v in zip(ring["names"], ring["vals"]):
                if v > 0:
                    toks.append((nm, 0, v))
        for e in self.ENG:
            for t in sorted(toks):
                self._wait(e, t)


def _consts():
    bf = ml_dtypes.bfloat16
    c = {}
    c["ident32"] = np.eye(128, dtype=np.float32)
    c["identbf"] = np.eye(128, dtype=np.float32).astype(bf)
    s = np.arange(128)
    same = np.ones((128, 128), dtype=bool)
    c["maskF"] = (same & (s[:, None] <= s[None, :])).astype(np.float32) * DH ** -0.5
    c["maskB"] = (same & (s[:, None] >= s[None, :])).astype(np.float32) * DH ** -0.5
    c["triincl"] = (same & (s[:, None] <= s[None, :])).astype(np.float32)
    c["blockones"] = same.astype(np.float32)
    c["sel0"] = np.repeat((s < 64).astype(np.float32)[:, None], 128, 1)
    c["sel1"] = np.repeat((s >= 64).astype(np.float32)[:, None], 128, 1)
    a = 2 * np.pi * np.outer(s, s) / 128.0
    c["ccs"] = np.concatenate([np.cos(a), -np.sin(a)], 1).astype(np.float32) * 128 ** -0.5
    c["ccs"] = c["ccs"].astype(bf)
    l = np.arange(L // 2, dtype=np.int64)
    sc = L ** -0.5
    tabC, tabS = [], []
    for e_ in range(2):
        for j in range(2):
            lp = 2 * (j * 512 + np.arange(512, dtype=np.int64)) + e_
            ang = 2 * np.pi * ((l[:, None] * lp[None, :]) % L).astype(np.float64) / L
            for tab, m in ((tabC, np.cos(ang) * sc), (tabS, np.sin(ang) * sc)):
                m = m.astype(np.float32).reshape(8, 128, 512)
                tab.append(np.ascontiguousarray(m.transpose(1, 0, 2)))
    c["dftC"] = np.stack(tabC).astype(bf)
    c["dftS"] = np.stack(tabS).astype(bf)
    return c


_CONST_CACHE = {}


def build(NB, dbg=(), skip_moe=False):
    nc = bass.Bass("TRN2", target_bir_lowering=False)
    es = ExitStack()
    P = Prog(nc, es)

    def din(name, shape, dt=F32):
        return nc.dram_tensor(name, list(shape), dt, kind="ExternalInput").ap()

    def dscr(name, shape, dt):
        return nc.dram_tensor(name, list(shape), dt, kind="Internal").ap()

    x_d = din("x", [NB, L, D])
    ctx_d = din("ctx", [NB, CL, D])
    cvec_d = din("cvec", [5, D])
    wada_d = din("w_ada", [D, 6 * D])
    bada_d = din("b_ada", [1, 6 * D])
    gvec_d = din("gvec", [3, D])
    win_d = din("w_in", [D, IN_COLS])
    bg_d = din("b_gates", [1, 16])
    wconv_d = din("w_conv", [9, D])
    wf_d = din("w_fourier", [512, D])
    wm_d = din("w_mlstm", [512, D])
    wo_d = din("w_out", [D, D])
    wr_d = din("w_router", [D, NE])
    wge_d = din("w_gate_e", [NE, D, D])
    wue_d = din("w_up_e", [NE, D, D])
    wde_d = din("w_down_e", [NE, D, D])
    cd = {}
    for nm, shp, dt in (("ident32", [128, 128], F32), ("identbf", [128, 128], BF16), ("maskF", [128, 128], F32),
                        ("maskB", [128, 128], F32), ("triincl", [128, 128], F32), ("blockones", [128, 128], F32),
                        ("sel0", [128, 128], F32), ("sel1", [128, 128], F32), ("ccs", [128, 256], BF16),
                        ("dftC", [4, 128, 8, 512], BF16), ("dftS", [4, 128, 8, 512], BF16)):
        cd[nm] = din(nm, shp, dt)
    out_d = nc.dram_tensor("out", [NB, L, D], F32, kind="ExternalOutput").ap()
    dbg_d = {}
    for nm, shp, dt in dbg:
        dbg_d[nm] = nc.dram_tensor("dbg_" + nm, list(shp), dt, kind="ExternalOutput").ap()

    mod_d = dscr("mod_d", [5, 6 * D], F32)
    winbf_d = dscr("winbf_d", [D, IN_COLS], BF16)
    wfold_d = dscr("wfold_d", [D, D], BF16)
    wmbf_d = dscr("wmbf_d", [512, D], BF16)
    woutbf_d = dscr("woutbf_d", [D, D], BF16)
    pq_d = dscr("pq_d", [8, 128, L], BF16)
    hf_d = dscr("hf_d", [L, 512], BF16)
    webf_d = [dscr(f"webf{m}", [NE, D, D], BF16) for m in range(3)]
    x1_d = [dscr(f"x1_d{i}", [L, D], F32) for i in range(NB)]
    x1n_d = [dscr(f"x1n_d{i}", [L, D], BF16) for i in range(NB)]

    uniq = [0]

    def sb(name, shape, dt, stack=None):
        uniq[0] += 1
        return (stack or es).enter_context(nc.sbuf_tensor(f"s{uniq[0]}_{name}", list(shape), dt))

    def ps(name, shape, dt, stack=None):
        uniq[0] += 1
        return (stack or es).enter_context(nc.psum_tensor(f"p{uniq[0]}_{name}", list(shape), dt))

    PSB = [ps(f"psb{i}", [128, 512], F32) for i in range(8)]

    def psk(i):
        return ("psb", i)

    ident32 = sb("ident32", [128, 128], F32)
    identbf = sb("identbf", [128, 128], BF16)
    maskF = sb("maskF", [128, 128], F32)
    maskB = sb("maskB", [128, 128], F32)
    triincl = sb("triincl", [128, 128], F32)
    blockones = sb("blockones", [128, 128], F32)
    sel0 = sb("sel0", [128, 128], F32)
    sel1 = sb("sel1", [128, 128], F32)
    ccs = sb("ccs", [128, 256], BF16)
    modT = sb("modT", [128, 48, 5], F32)
    gT = sb("gT", [128, 8, 3], F32)
    wcT = sb("wcT", [128, 8, 9], F32)
    scale1T = sb("scale1T", [128, 8, 5], F32)
    scale2T = sb("scale2T", [128, 8, 5], F32)
    wr_sb = sb("wr_sb", [128, 8, NE], F32)
    bg_sb = sb("bg_sb", [128, 16], F32)
    eps_sb = sb("eps_sb", [128, 1], F32)
    S32 = sb("S32", [128, 8, 130], F32)
    Sbf = sb("Sbf", [128, 8, 130], BF16)
    affT_all = sb("affT_all", [64, L], F32)
    wst = [sb(f"wst{i}", [128, 2, D], BF16) for i in range(2)]
    pre_src = (wge_d, wue_d, wde_d)
    pre_steps = [(e_, m, q) for e_ in range(NE) for m in range(3) for q in range(4)]
    pre_state = dict(k=0, pending=None)

    def prepass_step():
        k = pre_state["k"]
        if k < len(pre_steps):
            e_, m, q = pre_steps[k]
            i = k % 2
            P.dma("pool", wst[i][:], pre_src[m][e_, q * 256:(q + 1) * 256, :].rearrange("(k p) n -> p k n", p=128),
                  writes=[("wst", i)])
        if pre_state["pending"] is not None:
            e2, m2, q2, i2 = pre_state["pending"]
            P.dma("pool", webf_d[m2][e2, q2 * 256:(q2 + 1) * 256, :].rearrange("(k p) n -> p k n", p=128), wst[i2][:],
                  reads=[("wst", i2)], writes=[("webf", m2, e2)])
            pre_state["pending"] = None
        if k < len(pre_steps):
            pre_state["pending"] = (e_, m, q, i)
            pre_state["k"] = k + 1

    def prepass_done():
        return pre_state["k"] >= len(pre_steps) and pre_state["pending"] is None

    for nm, t in (("ident32", ident32), ("identbf", identbf), ("maskF", maskF), ("maskB", maskB),
                  ("triincl", triincl), ("blockones", blockones), ("sel0", sel0), ("sel1", sel1), ("ccs", ccs)):
        P.dma("sp", t[:], cd[nm], writes=[nm])
    P.dma("sp", wr_sb[:], wr_d.rearrange("(kc p) e -> p kc e", p=128), writes=["wr"])
    P.dma("sp", bg_sb[:], bg_d.partition_broadcast(128), writes=["bg"])
    P.op("dve", lambda e: e.memset(eps_sb[:], EPS), writes=["eps"])
    mhalf_sb = sb("mhalf_sb", [128, 1], F32)
    P.op("dve", lambda e: e.memset(mhalf_sb[:], -0.5), writes=["mhalf"])
    P.op("pool", lambda e: e.memset(affT_all[:], 0.0), writes=["affT_all"])

    def dbg_out(nm, src_ap, reads, dst=None):
        if nm in dbg_d:
            P.dma("sp", dst if dst is not None else dbg_d[nm], src_ap, reads=reads, writes=[("dbg", nm)])

    with ExitStack() as st:
        cv = sb("cv", [5, D], F32, st)
        csil = sb("csil", [5, D], F32, st)
        cT = sb("cT", [128, 8, 5], F32, st)
        modsb = sb("modsb", [5, 6 * D], F32, st)
        badasb = sb("badasb", [5, 6 * D], F32, st)
        wa = [sb(f"wa{i}", [128, 8, 512], F32, st) for i in range(2)]
        gv = sb("gv", [3, D], F32, st)
        wcv = sb("wcv", [9, D], F32, st)
        P.dma("sp", cv[:], cvec_d, writes=["cv"])
        P.dma("sp", badasb[:], bada_d.partition_broadcast(5), writes=["bada"])
        P.dma("sp", gv[:], gvec_d, writes=["gv"])
        P.dma("sp", wcv[:], wconv_d, writes=["wcv"])
        P.op("act", lambda e: e.activation(out=csil[:], in_=cv[:], func=AF.Silu), reads=["cv"], writes=["csil"])
        pA = PSB[0]
        for kc in range(8):
            P.op("pe", lambda e, kc=kc: e.transpose(out=pA[:, kc * 5:(kc + 1) * 5], in_=csil[0:5, kc * 128:(kc + 1) * 128],
                                                     identity=ident32[0:5, 0:5]),
                 reads=["csil", "ident32"], writes=[psk(0)], inc=(kc == 7))
        P.op("dve", lambda e: e.tensor_copy(out=cT[:].rearrange("p a b -> p (a b)"), in_=pA[:, 0:40]),
             reads=[psk(0)], writes=["cT"])
        for cb in range(12):
            w = wa[cb % 2]
            P.dma("sp" if cb % 2 == 0 else "act", w[:],
                  wada_d[:, cb * 512:(cb + 1) * 512].rearrange("(kc p) n -> p kc n", p=128), writes=[("wa", cb % 2)])
            pb = PSB[1 + cb % 2]
            for kc in range(8):
                P.op("pe", lambda e, kc=kc, w=w, pb=pb: e.matmul(pb[0:5, :], lhsT=cT[:, kc, :], rhs=w[:, kc, :],
                                                                   start=(kc == 0), stop=(kc == 7)),
                     reads=["cT", ("wa", cb % 2)], writes=[psk(1 + cb % 2)], inc=(kc == 7))
            P.op("dve", lambda e, cb=cb, pb=pb: e.tensor_tensor(out=modsb[:, cb * 512:(cb + 1) * 512], in0=pb[0:5, :],
                                                                in1=badasb[:, cb * 512:(cb + 1) * 512], op=ALU.add),
                 reads=[psk(1 + cb % 2), "bada"], writes=["modsb"])
        P.dma("sp", mod_d, modsb[:], reads=["modsb"], writes=["mod_d"])
        dbg_out("mod", modsb[:], ["modsb"])
        pA = PSB[3]
        for j in range(48):
            P.op("pe", lambda e, j=j: e.transpose(out=pA[:, j * 5:(j + 1) * 5], in_=modsb[0:5, j * 128:(j + 1) * 128],
                                                   identity=ident32[0:5, 0:5]),
                 reads=["modsb", "ident32"], writes=[psk(3)], inc=(j == 47))
        P.op("dve", lambda e: e.tensor_copy(out=modT[:].rearrange("p a b -> p (a b)"), in_=pA[:, 0:240]),
             reads=[psk(3)], writes=["modT"])
        pA = PSB[4]
        for kc in range(8):
            P.op("pe", lambda e, kc=kc: e.transpose(out=pA[:, kc * 3:(kc + 1) * 3], in_=gv[0:3, kc * 128:(kc + 1) * 128],
                                                     identity=ident32[0:3, 0:3]),
                 reads=["gv", "ident32"], writes=[psk(4)], inc=(kc == 7))
        P.op("dve", lambda e: e.tensor_copy(out=gT[:].rearrange("p a b -> p (a b)"), in_=pA[:, 0:24]),
             reads=[psk(4)], writes=["gT"])
        pA = PSB[5]
        for kc in range(8):
            P.op("pe", lambda e, kc=kc: e.transpose(out=pA[:, kc * 9:(kc + 1) * 9], in_=wcv[0:9, kc * 128:(kc + 1) * 128],
                                                     identity=ident32[0:9, 0:9]),
                 reads=["wcv", "ident32"], writes=[psk(5)], inc=(kc == 7))
        P.op("dve", lambda e: e.tensor_copy(out=wcT[:].rearrange("p a b -> p (a b)"), in_=pA[:, 0:72]),
             reads=[psk(5)], writes=["wcT"])
        for kc in range(8):
            P.op("dve", lambda e, kc=kc: e.tensor_scalar(out=scale1T[:, kc, :], in0=modT[:, 8 + kc, :], scalar1=1.0,
                                                          scalar2=gT[:, kc, 0:1], op0=ALU.add, op1=ALU.mult),
                 reads=["modT", "gT"], writes=["scale1T"])
            P.op("dve", lambda e, kc=kc: e.tensor_scalar(out=scale2T[:, kc, :], in0=modT[:, 32 + kc, :], scalar1=1.0,
                                                          scalar2=gT[:, kc, 1:2], op0=ALU.add, op1=ALU.mult),
                 reads=["modT", "gT"], writes=["scale2T"])
        wtmp = [sb(f"wtmp{i}", [128, 8, 512], BF16, st) for i in range(2)]
        ngr = (IN_COLS + 511) // 512
        for g in range(ngr):
            c0 = g * 512
            w_ = min(512, IN_COLS - c0)
            t = wtmp[g % 2]
            P.dma("pool", t[:, :, 0:w_], win_d[:, c0:c0 + w_].rearrange("(kc p) n -> p kc n", p=128),
                  writes=[("wtmp", g % 2)])
            P.dma("sp", winbf_d[:, c0:c0 + w_].rearrange("(kc p) n -> p kc n", p=128), t[:, :, 0:w_],
                  reads=[("wtmp", g % 2)], writes=["winbf_d"])
        wfbf = sb("wfbf", [128, 4, D], BF16, st)
        wfo = sb("wfo", [128, 8, D], BF16, st)
        P.dma("pool", wfbf[:], wf_d.rearrange("(g p) n -> p g n", p=128), writes=["wfbf"])
        i = 0
        for g in range(4):
            for m in range(2):
                for hf in range(2):
                    bk = 6 + (i % 2)
                    pb = PSB[bk]
                    P.op("pe", lambda e, g=g, m=m, hf=hf, pb=pb: e.matmul(pb[:, :], lhsT=ccs[:, m * 128:(m + 1) * 128],
                                                                           rhs=wfbf[:, g, hf * 512:(hf + 1) * 512],
                                                                           start=True, stop=True),
                         reads=["ccs", "wfbf"], writes=[psk(bk)])
                    P.op("act" if i % 2 == 0 else "dve",
                         (lambda e, g=g, m=m, hf=hf, pb=pb: e.activation(out=wfo[:, m * 4 + g, hf * 512:(hf + 1) * 512],
                                                                         in_=pb[:, :], func=AF.Copy)) if i % 2 == 0 else
                         (lambda e, g=g, m=m, hf=hf, pb=pb: e.tensor_copy(out=wfo[:, m * 4 + g, hf * 512:(hf + 1) * 512],
                                                                          in_=pb[:, :])),
                         reads=[psk(bk)], writes=["wfo"])
                    i += 1
        P.dma("sp", wfold_d.rearrange("(kc p) n -> p kc n", p=128), wfo[:], reads=["wfo"], writes=["wfold_d"])
        wcast = sb("wcast", [128, 12, D], BF16, st)
        P.dma("pool", wcast[:, 0:8, :], wo_d.rearrange("(kc p) n -> p kc n", p=128), writes=["wcast_o"])
        P.dma("pool", wcast[:, 8:12, :], wm_d.rearrange("(kc p) n -> p kc n", p=128), writes=["wcast_m"])
        P.dma("sp", woutbf_d.rearrange("(kc p) n -> p kc n", p=128), wcast[:, 0:8, :], reads=["wcast_o"], writes=["woutbf_d"])
        P.dma("sp", wmbf_d.rearrange("(kc p) n -> p kc n", p=128), wcast[:, 8:12, :], reads=["wcast_m"], writes=["wmbf_d"])
        P.barrier()

    bst = ExitStack()
    hmT = sb("hmT", [128, 4, L], BF16, bst)
    gates = gsm = dec = None

    def rms_rstd(eng_sq, xt, junk, ss, rt, rstd, kx, kss):
        P.op("dve", lambda e: e.scalar_tensor_tensor(out=junk, in0=xt, scalar=1.0, in1=xt, op0=ALU.mult, op1=ALU.mult,
                                                     accum_out=ss), reads=[kx], writes=[kss + "junk", kss])
        P.op("act", lambda e: e.activation(out=rt, in_=ss, func=AF.Sqrt, scale=1.0 / D, bias=eps_sb[:, 0:1]),
             reads=[kss, "eps"], writes=[kss + "rt"])
        P.op("dve", lambda e: e.reciprocal(out=rstd, in_=rt), reads=[kss + "rt"], writes=[kss + "rstd"])

    def norm_to_T(src_tile_ap, ntiles, r, dstT, kdst, st, tag):
        xt = [sb(f"xt{tag}{i}", [128, D], F32, st) for i in range(3)]
        junk = sb(f"junk{tag}", [128, D], BF16, st)
        xn = [sb(f"xn{tag}{i}", [128, D], BF16, st) for i in range(3)]
        sm = [sb(f"sm{tag}{i}", [128, 4], F32, st) for i in range(3)]
        psTs = [PSB[j][:].bitcast(BF16) for j in range(4)]
        for t in range(ntiles):
            i = t % 3
            ib = t % 2
            P.dma("sp", xt[i][:], src_tile_ap(t), writes=[("xt", tag, i)])
            rms_rstd("dve", xt[i][:], junk[:], sm[i][:, 0:1], sm[i][:, 1:2], sm[i][:, 2:3], ("xt", tag, i), f"ss{tag}{i}")
            P.op("act", lambda e, i=i: e.activation(out=xn[i][:], in_=xt[i][:], func=AF.Copy, scale=sm[i][:, 2:3]),
                 reads=[("xt", tag, i), f"ss{tag}{i}rstd"], writes=[("xn", tag, i)])
            for kc in range(8):
                bk = 2 * ib + kc // 4
                pT = psTs[bk]
                P.op("pe", lambda e, kc=kc, i=i, pT=pT: e.transpose(out=pT[:, (kc % 4) * 128:(kc % 4 + 1) * 128],
                                                                     in_=xn[i][:, kc * 128:(kc + 1) * 128],
                                                                     identity=identbf[:]),
                     reads=[("xn", tag, i), "identbf"], writes=[psk(bk)], inc=(kc % 4 == 3))
            for kc in range(8):
                bk = 2 * ib + kc // 4
                pT = psTs[bk]
                src = pT[:, (kc % 4) * 128:(kc % 4 + 1) * 128]
                if kc // 4 == 0:
                    P.op("act", lambda e, kc=kc, t=t, src=src: e.activation(out=dstT[:, kc, t * 128:(t + 1) * 128], in_=src,
                                                                            func=AF.Identity, scale=scale1T[:, kc, r:r + 1],
                                                                            bias=modT[:, kc, r:r + 1]),
                         reads=[psk(bk), "scale1T", "modT"], writes=[(kdst, kc)])
                else:
                    P.op("dve", lambda e, kc=kc, t=t, src=src: e.tensor_scalar(out=dstT[:, kc, t * 128:(t + 1) * 128], in0=src,
                                                                               scalar1=scale1T[:, kc, r:r + 1],
                                                                               scalar2=modT[:, kc, r:r + 1],
                                                                               op0=ALU.mult, op1=ALU.add),
                         reads=[psk(bk), "scale1T", "modT"], writes=[(kdst, kc)])

    def load_wcols(tile, c0, w_, key, q="sp"):
        P.dma(q, tile[:, :, 0:w_], winbf_d[:, c0:c0 + w_].rearrange("(kc p) n -> p kc n", p=128),
              reads=["winbf_d"], writes=[key])

    def proj_tok(hT, ntiles, wt, wkey, ncols, evac, banks, hkey="hT"):
        for t in range(ntiles):
            bk = banks[t % len(banks)]
            pb = PSB[bk]
            for kc in range(8):
                P.op("pe", lambda e, kc=kc, t=t, pb=pb: e.matmul(pb[:, 0:ncols], lhsT=hT[:, kc, t * 128:(t + 1) * 128],
                                                                  rhs=wt[:, kc, 0:ncols], start=(kc == 0), stop=(kc == 7)),
                     reads=[(hkey, kc), wkey], writes=[psk(bk)], inc=(kc == 7))
            evac(t, pb[:, 0:ncols], psk(bk))

    def gate_prep(t0, nt):
        sl = slice(t0, t0 + nt)
        ig, fl, C, T, U, G, W, tmp, tmp2 = (gsm[k] for k in ("ig", "fl", "C", "T", "u", "g", "w", "tmp", "tmp2"))
        gk = ("gsm", t0)
        g4 = gates[:, sl, :].rearrange("p t (k h) -> p t k h", k=4)
        ig4 = ig[:, sl, :].rearrange("p t (k h) -> p t k h", k=2)
        fl4 = fl[:, sl, :].rearrange("p t (k h) -> p t k h", k=2)
        tm4 = tmp[:, sl, :].rearrange("p t (k h) -> p t k h", k=2)
        for k in range(2):
            P.op("dve", lambda e, k=k: e.tensor_copy(out=ig4[:, :, k, :], in_=g4[:, :, 2 * k, :]),
                 reads=[("gates", t0)], writes=[gk])
            P.op("dve", lambda e, k=k: e.scalar_tensor_tensor(out=tm4[:, :, k, :], in0=g4[:, :, 2 * k + 1, :], scalar=-1.0,
                                                              in1=g4[:, :, 2 * k + 1, :], op0=ALU.mult, op1=ALU.max),
                 reads=[("gates", t0)], writes=[gk])
        P.op("act", lambda e: e.activation(out=tmp[:, sl, :], in_=tmp[:, sl, :], func=AF.Exp, scale=-1.0),
             reads=[gk], writes=[gk])
        P.op("act", lambda e: e.activation(out=tmp[:, sl, :], in_=tmp[:, sl, :], func=AF.Ln, bias=1.0),
             reads=[gk], writes=[gk])
        for k in range(2):
            P.op("dve", lambda e, k=k: e.scalar_tensor_tensor(out=fl4[:, :, k, :], in0=g4[:, :, 2 * k + 1, :], scalar=0.0,
                                                              in1=tm4[:, :, k, :], op0=ALU.min, op1=ALU.subtract),
                 reads=[("gates", t0), gk], writes=[gk])
        pC, pT_, pD0, pD1 = PSB[4], PSB[5], PSB[6], PSB[7]
        for j in range(nt):
            t = t0 + j
            for (mat, mk, pp, bk) in ((triincl, "triincl", pC, 4), (blockones, "blockones", pT_, 5)):
                P.op("pe", lambda e, mat=mat, pp=pp, j=j, t=t: e.matmul(pp[:, j * 8:(j + 1) * 8], lhsT=mat[:],
                                                                         rhs=fl[:, t, :], start=True, stop=True),
                     reads=[mk, gk], writes=[psk(bk)], inc=(j == nt - 1))
        n8 = nt * 8
        P.op("dve", lambda e: e.tensor_copy(out=C[:, sl, :].rearrange("p t k -> p (t k)"), in_=pC[:, 0:n8]),
             reads=[psk(4)], writes=[gk])
        P.op("dve", lambda e: e.tensor_copy(out=T[:, sl, :].rearrange("p t k -> p (t k)"), in_=pT_[:, 0:n8]),
             reads=[psk(5)], writes=[gk])
        P.op("act", lambda e: e.activation(out=dec[:, 0, sl, :], in_=T[:, sl, :], func=AF.Exp), reads=[gk], writes=[gk])
        f = slice(0, 4)
        bsl = slice(4, 8)

        def tt(out, a, b_, op):
            P.op("dve", lambda e: e.tensor_tensor(out=out, in0=a, in1=b_, op=op), reads=[gk], writes=[gk])

        tt(U[:, sl, f], ig[:, sl, f], C[:, sl, f], ALU.subtract)
        tt(G[:, sl, f], C[:, sl, f], C[:, sl, f], ALU.max)
        tt(W[:, sl, f], T[:, sl, f], U[:, sl, f], ALU.add)
        tt(tmp2[:, sl, bsl], T[:, sl, bsl], C[:, sl, bsl], ALU.subtract)
        tt(G[:, sl, bsl], tmp2[:, sl, bsl], fl[:, sl, bsl], ALU.add)
        tt(U[:, sl, bsl], ig[:, sl, bsl], G[:, sl, bsl], ALU.subtract)
        tt(tmp2[:, sl, bsl], C[:, sl, bsl], fl[:, sl, bsl], ALU.subtract)
        tt(W[:, sl, bsl], tmp2[:, sl, bsl], ig[:, sl, bsl], ALU.add)
        for X in (U, G, W):
            P.op("act", lambda e, X=X: e.activation(out=X[:, sl, :], in_=X[:, sl, :], func=AF.Exp), reads=[gk], writes=[gk])
        return gk

    for b in range(NB):
        P.barrier()
        with ExitStack() as sA:
            h1T = sb("h1T", [128, 8, L], BF16, sA)
            with ExitStack() as s1:
                norm_to_T(lambda t: x_d[b, t * 128:(t + 1) * 128, :], NT, b, h1T, "hT", s1, "a")
                if b == 0:
                    dbg_out("h1T", h1T[:], [("hT", kc) for kc in range(8)])
                wb = sb("wb_u", [128, 8, 512], BF16, s1)
                u_sb = sb("u_sb", [128, NT, 512], BF16, s1)
                load_wcols(wb, OFF_F, 512, "wb_u")

                def ev_u(t, pap, pk):
                    if t % 2 == 0:
                        P.op("act", lambda e: e.activation(out=u_sb[:, t, :], in_=pap, func=AF.Copy), reads=[pk], writes=[("u_sb", t)])
                    else:
                        P.op("dve", lambda e: e.tensor_copy(out=u_sb[:, t, :], in_=pap), reads=[pk], writes=[("u_sb", t)])
                proj_tok(h1T, NT, wb, "wb_u", 512, ev_u, [2, 3])
                dft = [sb(f"dft{i}", [128, 8, 512], BF16, s1) for i in range(3)]
                ueo = [sb(f"ueo{i}", [128, 8, 512], BF16, s1) for i in range(2)]
                pq2 = [sb(f"pq2{i}", [128, 1024], BF16, s1) for i in range(4)]
                for lc in range(8):
                    P.op("dve", lambda e, lc=lc: e.tensor_tensor(out=ueo[0][:, lc, :], in0=u_sb[:, lc, :], in1=u_sb[:, lc + 8, :], op=ALU.add),
                         reads=[("u_sb", lc), ("u_sb", lc + 8)], writes=[("ueo", 0, lc)])
                    P.op("pool", lambda e, lc=lc: e.tensor_tensor(out=ueo[1][:, lc, :], in0=u_sb[:, lc, :], in1=u_sb[:, lc + 8, :], op=ALU.subtract),
                         reads=[("u_sb", lc), ("u_sb", lc + 8)], writes=[("ueo", 1, lc)])
                it = 0
                dft_seq = [(j, m, nm, e_) for j in range(2) for m, nm in enumerate(("dftC", "dftS")) for e_ in range(2)]

                def dft_load(k):
                    if k < len(dft_seq):
                        j_, m_, nm_, ee_ = dft_seq[k]
                        P.dma("sp", dft[k % 3][:], cd[nm_][ee_ * 2 + j_], writes=[("dft", k % 3)])
                for k in range(3):
                    dft_load(k)
                for j in range(2):
                    for m, nm in enumerate(("dftC", "dftS")):
                        for e_ in range(2):
                            dt_ = dft[it % 3]
                            for cc in range(4):
                                bk = (it % 2) * 4 + cc
                                pb = PSB[bk]
                                for lc in range(8):
                                    P.op("pe", lambda e, cc=cc, lc=lc, pb=pb, dt_=dt_, e_=e_: e.matmul(
                                        pb[:, :], lhsT=ueo[e_][:, lc, cc * 128:(cc + 1) * 128], rhs=dt_[:, lc, :],
                                        start=(lc == 0), stop=(lc == 7)),
                                         reads=[("ueo", e_, lc), ("dft", it % 3)], writes=[psk(bk)], inc=(lc == 7))
                                q = pq2[cc][:].rearrange("p (n two) -> p n two", two=2)[:, :, e_]
                                if cc % 2 == 0:
                                    P.op("act", lambda e, q=q, pb=pb: e.activation(out=q, in_=pb[:, :], func=AF.Copy),
                                         reads=[psk(bk)], writes=[("pqs", cc)])
                                else:
                                    P.op("dve", lambda e, q=q, pb=pb: e.tensor_copy(out=q, in_=pb[:, :]),
                                         reads=[psk(bk)], writes=[("pqs", cc)])
                                if e_ == 1:
                                    P.dma("pool", pq_d[m * 4 + cc, :, j * 1024:(j + 1) * 1024], pq2[cc][:], reads=[("pqs", cc)],
                                          writes=["pq_d"])
                            dft_load(it + 3)
                            it += 1
                P.barrier()
            if b == 0:
                dbg_out("pq", pq_d, ["pq_d"])
            with ExitStack() as s2:
                gates = sb("gates", [128, NT + 2, 16], F32, s2)
                gsm = {nm: sb("g_" + nm, [128, NT + 2, 8], F32, s2) for nm in ("ig", "fl", "C", "T", "u", "g", "w", "tmp", "tmp2")}
                dec = sb("dec", [128, 2, NT + 2, 8], F32, s2)
                hcT = sb("hcT", [128, 8, CL], BF16, s2)
                qkT = sb("qkT", [128, 8, L], BF16, s2)
                kcT = sb("kcT", [128, 4, CL], BF16, s2)
                vex = sb("vex", [128, NT + 2, 4, 130], BF16, s2)
                s2a = ExitStack()
                norm_to_T(lambda t: ctx_d[b, t * 128:(t + 1) * 128, :], 2, 4, hcT, "hcT", s2a, "c")
                diag = sb("diag", [128, 72, 128], BF16, s2a)
                for kc in range(8):
                    for tap in range(9):
                        if (kc * 9 + tap) % 2 == 0:
                            P.op("dve", lambda e, kc=kc, tap=tap: e.tensor_scalar(out=diag[:, kc * 9 + tap, :], in0=ident32[:],
                                                                                  scalar1=wcT[:, kc, tap:tap + 1], scalar2=None,
                                                                                  op0=ALU.mult),
                                 reads=["ident32", "wcT"], writes=[("diag", kc)])
                        else:
                            P.op("act", lambda e, kc=kc, tap=tap: e.activation(out=diag[:, kc * 9 + tap, :], in_=ident32[:], func=AF.Copy,
                                                                               scale=wcT[:, kc, tap:tap + 1]),
                                 reads=["ident32", "wcT"], writes=[("diag", kc)])
                wbs = [sb(f"wbs{i}", [128, 8, 512], BF16, s2a) for i in range(2)]
                cin = [sb(f"cin{i}", [128, 66 + L + 66], BF16, s2a) for i in range(3)]
                for i in range(3):
                    P.op("dve", lambda e, i=i: e.memset(cin[i][:], 0.0), writes=[("cin", i)])
                P.op("dve", lambda e: e.memset(vex[:], 1.0), writes=["vex"])
                load_wcols(wbs[0], OFF_V, 512, ("wbs", 0))
                load_wcols(wbs[1], OFF_G, 16, ("wbs", 1), q="act")

                def ev_v(off):
                    def f(t, pap, pk):
                        o = vex[:, off + t, :, 0:128]
                        i_ = pap.rearrange("p (h d) -> p h d", h=4)
                        if t % 2 == 0:
                            P.op("act", lambda e: e.activation(out=o, in_=i_, func=AF.Copy), reads=[pk], writes=["vex"])
                        else:
                            P.op("dve", lambda e: e.tensor_copy(out=o, in_=i_), reads=[pk], writes=["vex"])
                    return f

                def ev_g(off):
                    def f(t, pap, pk):
                        P.op("dve", lambda e: e.tensor_tensor(out=gates[:, off + t, :], in0=pap, in1=bg_sb[:], op=ALU.add),
                             reads=[pk, "bg"], writes=[("gates", off)])
                    return f
                proj_tok(h1T, NT, wbs[0], ("wbs", 0), 512, ev_v(0), [0, 1])
                proj_tok(hcT, 2, wbs[0], ("wbs", 0), 512, ev_v(NT), [0, 1], hkey="hcT")
                proj_tok(h1T, NT, wbs[1], ("wbs", 1), 16, ev_g(0), [2, 3])
                proj_tok(hcT, 2, wbs[1], ("wbs", 1), 16, ev_g(NT), [2, 3], hkey="hcT")
                if b == 0:
                    dbg_out("gates", gates[:], [("gates", 0), ("gates", NT)])
                for grp in range(2):
                    w_ = wbs[grp % 2]
                    load_wcols(w_, OFF_QK + grp * 512, 512, ("wbs", grp % 2), q="sp")
                    for c4 in range(4):
                        cc = grp * 4 + c4
                        srcs = [("lat", h1T, L)] + ([("ctx", hcT, CL)] if grp == 1 else [])
                        for (kind, hT_, ntok) in srcs:
                            nblk = max(1, ntok // 512)
                            nn = min(512, ntok)
                            for nb_ in range(nblk):
                                pb = PSB[nb_]
                                for kc in range(8):
                                    P.op("pe", lambda e, kc=kc, nb_=nb_, pb=pb, hT_=hT_, w_=w_, c4=c4, nn=nn: e.matmul(
                                        pb[:, 0:nn], lhsT=w_[:, kc, c4 * 128:(c4 + 1) * 128],
                                        rhs=hT_[:, kc, nb_ * 512:nb_ * 512 + nn], start=(kc == 0), stop=(kc == 7)),
                                         reads=[("hT", kc), ("hcT", kc), ("wbs", grp % 2)], writes=[psk(nb_)], inc=(kc == 7))
                                P.op("act", lambda e, nb_=nb_, pb=pb, nn=nn: e.activation(
                                    out=cin[0][:, 66 + nb_ * 512:66 + nb_ * 512 + nn], in_=pb[:, 0:nn], func=AF.Copy),
                                     reads=[psk(nb_)], writes=[("cin", 0)])
                                if kind == "lat":
                                    for i_ in (1, 2):
                                        P.op("act", lambda e, nb_=nb_, pb=pb, i_=i_: e.activation(
                                            out=cin[i_][:, 66 + nb_ * 512:66 + nb_ * 512 + 512], in_=pb[:, 0:512], func=AF.Copy),
                                             reads=[psk(nb_)], writes=[("cin", i_)])
                            if kind == "lat":
                                for i_, xz in ((1, 63), (2, 0)):
                                    P.op("dve", lambda e, i_=i_, xz=xz: e.memset(
                                        cin[i_][:, 66:66 + L].rearrange("p (r x) -> p r x", x=64)[:, :, xz:xz + 1], 0.0),
                                         writes=[("cin", i_)])
                            if kind == "lat":
                                taps = [(dy, dx) for dy in range(3) for dx in range(3)]
                            else:
                                P.op("pool", lambda e: e.memset(cin[0][:, 66 + CL:66 + CL + 2], 0.0), writes=[("cin", 0)])
                                taps = [(1, dx) for dx in range(3)]
                            for nb_ in range(nblk):
                                pb = PSB[4 + nb_]
                                for ti, (dy, dx) in enumerate(taps):
                                    src = cin[{0: 1, 1: 0, 2: 2}[dx]] if kind == "lat" else cin[0]
                                    sh = 64 * (dy - 1) + (dx - 1)
                                    o0 = 66 + nb_ * 512 + sh
                                    P.op("pe", lambda e, pb=pb, src=src, o0=o0, nn=nn, dy=dy, dx=dx, ti=ti, cc=cc, taps=taps: e.matmul(
                                        pb[:, 0:nn], lhsT=diag[:, cc * 9 + dy * 3 + dx, :], rhs=src[:, o0:o0 + nn],
                                        start=(ti == 0), stop=(ti == len(taps) - 1)),
                                         reads=[("diag", cc), ("cin", 0), ("cin", 1), ("cin", 2)], writes=[psk(4 + nb_)],
                                         inc=(ti == len(taps) - 1))
                                dst = qkT[:, cc, nb_ * 512:nb_ * 512 + nn] if kind == "lat" else kcT[:, c4, 0:nn]
                                P.op("act", lambda e, pb=pb, dst=dst, nn=nn: e.activation(out=dst, in_=pb[:, 0:nn], func=AF.Silu),
                                     reads=[psk(4 + nb_)], writes=["qkT" if kind == "lat" else "kcT"])
                            if kind == "ctx":
                                pass
                if b == 0:
                    dbg_out("qkT", qkT[:], ["qkT"])
                    dbg_out("kcT", kcT[:], ["kcT"])
                P.barrier()
                s2a.close()
                gk_l = gate_prep(0, NT)
                gk_c = gate_prep(NT, 2)
                if b == 0:
                    for nm in ("u", "g", "w"):
                        dbg_out("gs_" + nm, gsm[nm][:], [gk_l, gk_c])
                    dbg_out("dec", dec[:], [gk_l, gk_c])
                P.op("dve", lambda e: e.memset(S32[:], 0.0), writes=[("S", i) for i in range(8)])
                P.op("pool", lambda e: e.memset(Sbf[:], 0.0), writes=[("Sbf", i) for i in range(8)])
                NR = 4
                kt = [sb(f"kt{i}", [128, 128], BF16, s2) for i in range(NR)]
                vt = [sb(f"vt{i}", [128, 130], BF16, s2) for i in range(NR)]
                vw = [sb(f"vw{i}", [128, 130], BF16, s2) for i in range(NR)]
                smt = [sb(f"smt{i}", [128, 128], BF16, s2) for i in range(NR)]
                hsm = [sb(f"hsm{i}", [128, 4], F32, s2) for i in range(NR)]
                hbuf = sb("hbuf", [128, NT, 512], BF16, s2)
                hsum = [sb(f"hsum{i}", [128, 512], F32, s2) for i in range(2)]
                ogs = [sb(f"ogs{i}", [128, 512], F32, s2) for i in range(2)]
                hmt = [sb(f"hmt{i}", [128, 512], BF16, s2) for i in range(2)]
                wog = sb("wog", [128, 8, 512], BF16, s2)
                load_wcols(wog, OFF_O, 512, "wog", q="act")
                rr = [0, 0]

                units = []

                def add_tile(di, tg, kTsrc, qTsrc, tcol, outputs, lt, first):
                    for h in range(4):
                        units.append(dict(di=di, tg=tg, kTsrc=kTsrc, qTsrc=qTsrc, tcol=tcol, outputs=outputs, lt=lt, first=first, h=h))

                def stage1(u, k):
                    di, tg, h, tcol, kTsrc, qTsrc = u["di"], u["tg"], u["h"], u["tcol"], u["kTsrc"], u["qTsrc"]
                    if h == 0:
                        prepass_step()
                        u["fi"] = rr[1] % 2
                        if u["outputs"] and not u["first"]:
                            rr[1] += 1
                    else:
                        u["fi"] = units[k - 1]["fi"]
                    r_ = k % NR
                    u["r"] = r_
                    gi = di * 4 + h
                    pk_ = PSB[0][:].bitcast(BF16)[:, r_ * 128:(r_ + 1) * 128]
                    P.op("pe", lambda e: e.transpose(out=pk_, in_=kTsrc[:, h, tcol:tcol + 128], identity=identbf[:]),
                         reads=["qkT", "kcT", "identbf"], writes=[("ps0", r_)])
                    P.op("act", lambda e: e.activation(out=kt[r_][:], in_=pk_, func=AF.Copy, scale=DH ** -0.5),
                         reads=[("ps0", r_)], writes=[("kt", r_)])
                    P.op("act", lambda e: e.activation(out=vw[r_][:, 0:130], in_=vex[:, tg, h, 0:130], func=AF.Copy,
                                                       scale=gsm["w"][:, tg, gi:gi + 1]),
                         reads=["vex", ("gsm", 0), ("gsm", NT)], writes=[("vw", r_)])
                    if u["outputs"]:
                        P.op("act", lambda e: e.activation(out=vt[r_][:, 0:130], in_=vex[:, tg, h, 0:130], func=AF.Copy,
                                                           scale=gsm["u"][:, tg, gi:gi + 1]),
                             reads=["vex", ("gsm", 0), ("gsm", NT)], writes=[("vt", r_)])
                        pS = PSB[1][:, r_ * 128:(r_ + 1) * 128]
                        P.op("pe", lambda e: e.matmul(pS, lhsT=kTsrc[:, h, tcol:tcol + 128], rhs=qTsrc[:, h, tcol:tcol + 128],
                                                      start=True, stop=True),
                             reads=["qkT"], writes=[("ps1", r_)])
                        mk = maskF if di == 0 else maskB
                        P.op("dve", lambda e: e.tensor_tensor(out=smt[r_][:], in0=pS, in1=mk[:], op=ALU.mult),
                             reads=[("ps1", r_), "maskF", "maskB"], writes=[("smt", r_)])

                def stage2(u, k):
                    di, tg, h, tcol, qTsrc, r_ = u["di"], u["tg"], u["h"], u["tcol"], u["qTsrc"], u["r"]
                    sidx = di * 4 + h
                    gi = di * 4 + h
                    if u["outputs"]:
                        pO = PSB[2 + (r_ % 2)][:, (r_ // 2) * 256:(r_ // 2) * 256 + 129]
                        kO = ("pO", r_)
                        u["pO"], u["kO"] = pO, kO
                        P.op("pe", lambda e: e.matmul(pO, lhsT=smt[r_][:], rhs=vt[r_][:, 0:129], start=True, stop=False, skip_group_check=True),
                             reads=[("smt", r_), ("vt", r_)], writes=[kO], inc=False)
                        P.op("pe", lambda e: e.matmul(pO, lhsT=qTsrc[:, h, tcol:tcol + 128], rhs=Sbf[:, sidx, 0:129],
                                                      start=False, stop=True, skip_group_check=True),
                             reads=["qkT", ("Sbf", sidx)], writes=[kO])
                    bu = 4 + (k % 2)
                    pU = PSB[bu][:, 0:129]
                    kU = psk(bu)
                    P.op("pe", lambda e: e.matmul(pU, lhsT=kt[r_][:, :], rhs=vw[r_][:, 0:129], start=True, stop=True),
                         reads=[("kt", r_), ("vw", r_)], writes=[kU])
                    P.op("dve", lambda e: e.scalar_tensor_tensor(
                        out=S32[:, sidx, 0:129], in0=S32[:, sidx, 0:129], scalar=dec[:, 0, tg, gi:gi + 1], in1=pU,
                        op0=ALU.mult, op1=ALU.add),
                         reads=[kU, ("S", sidx), ("gsm", 0), ("gsm", NT)], writes=[("S", sidx)])
                    P.op("act", lambda e: e.activation(out=Sbf[:, sidx, 0:130], in_=S32[:, sidx, 0:130], func=AF.Copy),
                         reads=[("S", sidx)], writes=[("Sbf", sidx)])

                def stage3(u, k):
                    if not u["outputs"]:
                        return
                    di, tg, h, r_, lt, fi = u["di"], u["tg"], u["h"], u["r"], u["lt"], u["fi"]
                    gi = di * 4 + h
                    pO, kO = u["pO"], u["kO"]
                    hs = hsm[r_]
                    gcol = gsm["g"][:, tg, gi:gi + 1]
                    P.op("dve", lambda e: e.tensor_scalar(out=hs[:, 3:4], in0=pO[:, 128:129], scalar1=gcol, scalar2=None, op0=ALU.mult),
                         reads=[kO, ("gsm", 0)], writes=[("hsm", r_)])
                    P.op("dve", lambda e: e.scalar_tensor_tensor(out=hs[:, 0:1], in0=hs[:, 3:4], scalar=-1.0, in1=hs[:, 3:4],
                                                                 op0=ALU.mult, op1=ALU.max),
                         reads=[("hsm", r_)], writes=[("hsm", r_)])
                    P.op("dve", lambda e: e.tensor_scalar(out=hs[:, 0:1], in0=hs[:, 0:1], scalar1=1.0, scalar2=None, op0=ALU.max),
                         reads=[("hsm", r_)], writes=[("hsm", r_)])
                    P.op("dve", lambda e: e.reciprocal(out=hs[:, 1:2], in_=hs[:, 0:1]), reads=[("hsm", r_)], writes=[("hsm", r_)])
                    P.op("dve", lambda e: e.tensor_tensor(out=hs[:, 2:3], in0=hs[:, 1:2], in1=gcol, op=ALU.mult),
                         reads=[("hsm", r_), ("gsm", 0)], writes=[("hsm", r_)])
                    if u["first"]:
                        P.op("dve", lambda e: e.tensor_scalar(out=hbuf[:, lt, h * 128:(h + 1) * 128], in0=pO[:, 0:128],
                                                              scalar1=hs[:, 2:3], scalar2=None, op0=ALU.mult),
                             reads=[kO, ("hsm", r_)], writes=[("hbuf", lt)])
                    else:
                        P.op("dve", lambda e: e.scalar_tensor_tensor(
                            out=hsum[fi][:, h * 128:(h + 1) * 128], in0=pO[:, 0:128], scalar=hs[:, 2:3],
                            in1=hbuf[:, lt, h * 128:(h + 1) * 128], op0=ALU.mult, op1=ALU.add),
                             reads=[kO, ("hsm", r_), ("hbuf", lt)], writes=[("hsum", fi)])
                    if h == 3 and not u["first"]:
                        t = lt
                        pb = PSB[6 + fi]
                        for kc in range(8):
                            P.op("pe", lambda e, kc=kc: e.matmul(pb[:, :], lhsT=h1T[:, kc, t * 128:(t + 1) * 128], rhs=wog[:, kc, :],
                                                                  start=(kc == 0), stop=(kc == 7)),
                                 reads=[("hT", kc), "wog"], writes=[psk(6 + fi)], inc=(kc == 7))
                        P.op("act", lambda e: e.activation(out=ogs[fi][:], in_=pb[:, :], func=AF.Sigmoid),
                             reads=[psk(6 + fi)], writes=[("ogs", fi)])
                        P.op("pool", lambda e: e.tensor_tensor(out=hmt[fi][:], in0=hsum[fi][:], in1=ogs[fi][:], op=ALU.mult),
                             reads=[("hsum", fi), ("ogs", fi)], writes=[("hmt", fi)])
                        pT = PSB[6 + fi][:].bitcast(BF16)
                        for h2 in range(4):
                            P.op("pe", lambda e, h2=h2: e.transpose(out=pT[:, h2 * 128:(h2 + 1) * 128], in_=hmt[fi][:, h2 * 128:(h2 + 1) * 128],
                                                                    identity=identbf[:]),
                                 reads=[("hmt", fi), "identbf"], writes=[psk(6 + fi)], inc=(h2 == 3))
                        P.op("act", lambda e: e.activation(out=hmT[:, :, t * 128:(t + 1) * 128],
                                                           in_=pT[:, 0:512].rearrange("p (h n) -> p h n", h=4), func=AF.Copy),
                             reads=[psk(6 + fi)], writes=["hmT"])

                for i in range(2):
                    add_tile(0, NT + i, kcT, kcT, i * 128, False, i, True)
                    add_tile(1, NT + 1 - i, kcT, kcT, (1 - i) * 128, False, 1 - i, True)
                for s_ in range(NT):
                    tf, tb = s_, NT - 1 - s_
                    first = s_ < NT // 2
                    add_tile(0, tf, qkT[:, 4:8, :], qkT[:, 0:4, :], tf * 128, True, tf, first)
                    add_tile(1, tb, qkT[:, 4:8, :], qkT[:, 0:4, :], tb * 128, True, tb, first)
                stage1(units[0], 0)
                stage1(units[1], 1)
                for k, u in enumerate(units):
                    stage2(u, k)
                    if k + 2 < len(units):
                        stage1(units[k + 2], k + 2)
                    stage3(u, k)
                    if b == 0 and k == 15:
                        dbg_out("S_b", S32[:], [("S", i) for i in range(8)], dst=dbg_d.get("S_b"))
                if b == 0:
                    dbg_out("S_f", S32[:], [("S", i) for i in range(8)])
                if b == 0:
                    dbg_out("hmT", hmT[:], ["hmT"])
                P.barrier()
            with ExitStack() as s3:
                wfold = sb("wfold", [128, 8, D], BF16, s3)
                wm = sb("wm", [128, 4, D], BF16, s3)
                wout = sb("wout", [128, 8, D], BF16, s3)
                wbr = sb("wbr", [128, 8, 2048], BF16, s3)
                ga1 = sb("ga1", [128, D], F32, s3)
                pqb = sb("pqb", [128, 8, 512], BF16, s3)
                mergedT = sb("mergedT", [128, 8, 512], BF16, s3)
                sgF2 = [sb(f"sgF{i}", [128, 512], F32, s3) for i in range(2)]
                sgM2 = [sb(f"sgM{i}", [128, 512], F32, s3) for i in range(2)]
                m12 = [sb(f"m1{i}", [128, 512], F32, s3) for i in range(2)]
                xt2 = [sb("xt2_0", [128, D], F32, s3)] * 2
                x1t = [sb(f"x1t{i}", [128, D], F32, s3) for i in range(2)]
                x1n = sb("x1n", [128, D], F32, s3)
                x1nb = sb("x1nb", [128, D], BF16, s3)
                junk3 = sb("junk3", [128, D], BF16, s3)
                hx2T = sb("hx2T", [128, 8, 128], F32, s3)
                sm3 = sb("sm3", [128, 8], F32, s3)
                sm3x = [sb(f"sm3x{i}", [128, 4], F32, s3) for i in range(2)]
                esb = sb("esb", [128, 16], F32, s3)
                aff = sb("aff", [128, 16], F32, s3)
                afft = [sb(f"afft{i}", [16, 128], F32, s3) for i in range(2)]
                P.dma("pool", pqb[:], pq_d[:, :, 0:512].rearrange("k p n -> p k n"), reads=["pq_d"], writes=["pqb"])
                for hf_ in range(2):
                    cs_ = slice(hf_ * 512, (hf_ + 1) * 512)
                    P.dma("sp", wfold[:, :, cs_], wfold_d[:, cs_].rearrange("(kc p) n -> p kc n", p=128), reads=["wfold_d"],
                          writes=[("wfold", hf_)])
                    P.dma("act", wm[:, :, cs_], wmbf_d[:, cs_].rearrange("(kc p) n -> p kc n", p=128), reads=["wmbf_d"],
                          writes=[("wm", hf_)])
                    for g_ in range(2 * hf_, 2 * hf_ + 2):
                        for fm in range(2):
                            c0_ = fm * 1024 + g_ * 256
                            P.dma("act" if fm == 0 else "sp", wbr[:, :, c0_:c0_ + 256],
                                  winbf_d[:, OFF_BR + c0_:OFF_BR + c0_ + 256].rearrange("(kc p) n -> p kc n", p=128),
                                  reads=["winbf_d"], writes=[("wbr", g_)])
                P.dma("act", wout[:], woutbf_d.rearrange("(kc p) n -> p kc n", p=128), reads=["woutbf_d"], writes=["wout"])
                P.dma("sp", ga1[:], mod_d[b:b + 1, 2 * D:3 * D].partition_broadcast(128), reads=["mod_d"], writes=["ga1"])
                for kc in range(8):
                    if kc % 2 == 0:
                        P.op("dve", lambda e, kc=kc: e.tensor_tensor(out=wout[:, kc, :], in0=wout[:, kc, :], in1=ga1[:], op=ALU.mult),
                             reads=["wout", "ga1"], writes=["wout"])
                    else:
                        P.op("pool", lambda e, kc=kc: e.tensor_tensor(out=wout[:, kc, :], in0=wout[:, kc, :], in1=ga1[:], op=ALU.mult),
                             reads=["wout", "ga1"], writes=["wout"])
                for nb_ in range(4):
                    tsl = slice(nb_ * 512, (nb_ + 1) * 512)
                    for dc in range(8):
                        dsl = slice(dc * 128, (dc + 1) * 128)
                        p4 = (dc % 2) * 4
                        sF, sM, m1_ = sgF2[dc % 2], sgM2[dc % 2], m12[dc % 2]
                        kF, kM, k1 = ("sgF", dc % 2), ("sgM", dc % 2), ("m1", dc % 2)
                        for kc in range(8):
                            P.op("pe", lambda e, kc=kc, dsl=dsl, p4=p4: e.matmul(PSB[p4][:, :], lhsT=wfold[:, kc, dsl], rhs=pqb[:, kc, :],
                                                                                  start=(kc == 0), stop=(kc == 7)),
                                 reads=[("wfold", dc // 4), "pqb"], writes=[psk(p4)], inc=(kc == 7))
                        for kc in range(4):
                            P.op("pe", lambda e, kc=kc, dsl=dsl, tsl=tsl, p4=p4: e.matmul(PSB[p4 + 1][:, :], lhsT=wm[:, kc, dsl], rhs=hmT[:, kc, tsl],
                                                                                           start=(kc == 0), stop=(kc == 3)),
                                 reads=[("wm", dc // 4), "hmT"], writes=[psk(p4 + 1)], inc=(kc == 3))
                        for gi_, bk in ((0, p4 + 2), (1, p4 + 3)):
                            for kc in range(8):
                                P.op("pe", lambda e, kc=kc, dc=dc, tsl=tsl, gi_=gi_, bk=bk: e.matmul(
                                    PSB[bk][:, :], lhsT=wbr[:, kc, gi_ * 1024 + dc * 128:gi_ * 1024 + (dc + 1) * 128],
                                    rhs=h1T[:, kc, tsl], start=(kc == 0), stop=(kc == 7)),
                                     reads=[("wbr", dc // 2), ("hT", kc)], writes=[psk(bk)], inc=(kc == 7))
                        P.op("act", lambda e, sF=sF, p4=p4: e.activation(out=sF[:], in_=PSB[p4 + 2][:, :], func=AF.Sigmoid), reads=[psk(p4 + 2)], writes=[kF])
                        P.op("act", lambda e, sM=sM, p4=p4: e.activation(out=sM[:], in_=PSB[p4 + 3][:, :], func=AF.Sigmoid), reads=[psk(p4 + 3)], writes=[kM])
                        P.op("dve", lambda e, sF=sF, m1_=m1_, p4=p4: e.tensor_tensor(out=m1_[:], in0=PSB[p4][:, :], in1=sF[:], op=ALU.mult),
                             reads=[psk(p4), kF], writes=[k1])
                        P.op("dve", lambda e, sM=sM, p4=p4: e.tensor_tensor(out=sM[:], in0=PSB[p4 + 1][:, :], in1=sM[:], op=ALU.mult),
                             reads=[psk(p4 + 1), kM], writes=[kM])
                        P.op("pool", lambda e, dc=dc, m1_=m1_, sM=sM: e.tensor_tensor(out=mergedT[:, dc, :], in0=m1_[:], in1=sM[:], op=ALU.add),
                             reads=[k1, kM], writes=["mergedT"])
                    if nb_ + 1 < 4:
                        P.dma("sp", pqb[:], pq_d[:, :, (nb_ + 1) * 512:(nb_ + 2) * 512].rearrange("k p n -> p k n"),
                              reads=["pq_d"], writes=["pqb"])

                    def X1(tt):
                        t = nb_ * 4 + tt
                        i = t % 2
                        rows = slice(t * 128, (t + 1) * 128)
                        prepass_step()
                        P.dma("act", xt2[i][:], x_d[b, rows, :], writes=[("xt2", 0)])
                        for half in range(2):
                            hs_ = slice(half * 512, (half + 1) * 512)
                            for kc in range(8):
                                P.op("pe", lambda e, kc=kc, half=half, hs_=hs_: e.matmul(
                                    PSB[4 + half][:, :], lhsT=mergedT[:, kc, tt * 128:(tt + 1) * 128], rhs=wout[:, kc, hs_],
                                    start=(kc == 0), stop=(kc == 7)),
                                     reads=["mergedT", "wout"], writes=[psk(4 + half)], inc=(kc == 7))
                            P.op("dve", lambda e, half=half, hs_=hs_: e.tensor_tensor(out=x1t[i][:, hs_], in0=PSB[4 + half][:, :],
                                                                                      in1=xt2[i][:, hs_], op=ALU.add),
                                 reads=[psk(4 + half), ("xt2", 0)], writes=[("x1t", i)])
                        P.dma("sp", x1_d[b][rows, :], x1t[i][:], reads=[("x1t", i)], writes=[("x1_d", b)])
                        P.op("dve", lambda e: e.scalar_tensor_tensor(out=junk3[:], in0=x1t[i][:], scalar=1.0, in1=x1t[i][:], op0=ALU.mult,
                                                                     op1=ALU.mult, accum_out=sm3x[i][:, 0:1]),
                             reads=[("x1t", i)], writes=[f"ss3{i}junk", f"ss3{i}"])
                        P.op("dve", lambda e: e.tensor_scalar(out=sm3x[i][:, 1:2], in0=sm3x[i][:, 0:1], scalar1=1.0 / D, scalar2=EPS,
                                                              op0=ALU.mult, op1=ALU.add),
                             reads=[f"ss3{i}"], writes=[f"ss3{i}rt"])
                        P.op("pool", lambda e: e.tensor_tensor(out=sm3x[i][:, 2:3], in0=sm3x[i][:, 1:2], in1=mhalf_sb[:, 0:1], op=ALU.pow),
                             reads=[f"ss3{i}rt", "mhalf"], writes=[f"ss3{i}rstd"])

                    def X2(tt):
                        t = nb_ * 4 + tt
                        i = t % 2
                        rows = slice(t * 128, (t + 1) * 128)
                        P.op("dve", lambda e: e.tensor_scalar(out=x1n[:], in0=x1t[i][:], scalar1=sm3x[i][:, 2:3], scalar2=None, op0=ALU.mult),
                             reads=[("x1t", i), f"ss3{i}rstd"], writes=["x1n"])
                        P.op("act", lambda e: e.activation(out=x1nb[:], in_=x1t[i][:], func=AF.Copy, scale=sm3x[i][:, 2:3]),
                             reads=[("x1t", i), f"ss3{i}rstd"], writes=["x1nb"])
                        P.dma("sp", x1n_d[b][rows, :], x1nb[:], reads=["x1nb"], writes=[("x1n_d", b)])

                    def Y(tt):
                        t = nb_ * 4 + tt
                        i = t % 2
                        rows = slice(t * 128, (t + 1) * 128)
                        for kc in range(8):
                            bk = 6 + kc // 4
                            P.op("pe", lambda e, kc=kc, bk=bk: e.transpose(out=PSB[bk][:, (kc % 4) * 128:(kc % 4 + 1) * 128],
                                                                           in_=x1n[:, kc * 128:(kc + 1) * 128], identity=ident32[:]),
                                 reads=["x1n", "ident32"], writes=[psk(bk)], inc=(kc % 4 == 3))
                        for kc in range(8):
                            bk = 6 + kc // 4
                            src = PSB[bk][:, (kc % 4) * 128:(kc % 4 + 1) * 128]
                            if bk == 6:
                                P.op("act", lambda e, kc=kc, src=src: e.activation(out=hx2T[:, kc, :], in_=src, func=AF.Identity,
                                                                                   scale=scale2T[:, kc, b:b + 1], bias=modT[:, 24 + kc, b:b + 1]),
                                     reads=[psk(bk), "scale2T", "modT"], writes=[("hx2T", kc)])
                            else:
                                P.op("dve", lambda e, kc=kc, src=src: e.tensor_scalar(out=hx2T[:, kc, :], in0=src, scalar1=scale2T[:, kc, b:b + 1],
                                                                                      scalar2=modT[:, 24 + kc, b:b + 1], op0=ALU.mult, op1=ALU.add),
                                     reads=[psk(bk), "scale2T", "modT"], writes=[("hx2T", kc)])
                        for kc in range(8):
                            P.op("pe", lambda e, kc=kc: e.matmul(PSB[0][:, 0:NE], lhsT=hx2T[:, kc, :], rhs=wr_sb[:, kc, :],
                                                                  start=(kc == 0), stop=(kc == 7)),
                                 reads=[("hx2T", kc), "wr"], writes=[psk(0)], inc=(kc == 7))
                        P.op("dve", lambda e: e.tensor_reduce(out=sm3[:, 3:4], in_=PSB[0][:, 0:NE], axis=mybir.AxisListType.X, op=ALU.max),
                             reads=[psk(0)], writes=["sm3mx"])
                        P.op("dve", lambda e: e.tensor_scalar(out=sm3[:, 4:5], in0=sm3[:, 3:4], scalar1=-1.0, scalar2=None, op0=ALU.mult),
                             reads=["sm3mx"], writes=["sm3nmx"])
                        P.op("act", lambda e: e.activation(out=esb[:], in_=PSB[0][:, 0:NE], func=AF.Exp, bias=sm3[:, 4:5], accum_out=sm3[:, 5:6]),
                             reads=[psk(0), "sm3nmx"], writes=["esb", "sm3sum"])
                        P.op("dve", lambda e: e.reciprocal(out=sm3[:, 6:7], in_=sm3[:, 5:6]), reads=["sm3sum"], writes=["sm3r"])
                        P.op("dve", lambda e: e.tensor_scalar(out=aff[:], in0=esb[:], scalar1=sm3[:, 6:7], scalar2=None, op0=ALU.mult),
                             reads=["esb", "sm3r"], writes=["aff"])
                        P.op("pe", lambda e: e.transpose(out=PSB[1][0:NE, 0:128], in_=aff[:, :], identity=ident32[:]),
                             reads=["aff", "ident32"], writes=[psk(1)])
                        P.op("dve", lambda e: e.tensor_copy(out=afft[i][:], in_=PSB[1][0:NE, 0:128]), reads=[psk(1)], writes=[("afft", i)])
                        P.dma("sp", affT_all[b * NE:(b + 1) * NE, rows], afft[i][:], reads=[("afft", i)], writes=["affT_all"])

                    X1(0)
                    X2(0)
                    for tt in range(4):
                        if tt + 1 < 4:
                            X1(tt + 1)
                        Y(tt)
                        if tt + 1 < 4:
                            X2(tt + 1)
                if b == 0:
                    dbg_out("x1", x1_d[0], [("x1_d", 0)])
                    dbg_out("affT", affT_all[0:NE, :], ["affT_all"])
                P.barrier()
    bst.close()
    P.barrier()
    NP = NB * NE
    NS = NB * CAP
    if skip_moe:
        es.close()
        return nc
    with ExitStack() as sm:
        vals = sb("vals", [NP, CAP], F32, sm)
        idxu = sb("idxu", [NP, CAP], U32, sm)
        idxf = sb("idxf", [NP, CAP], F32, sm)
        idxT = sb("idxT", [128, 2, 64], I32, sm)
        valsT = sb("valsT", [128, 2, 64], F32, sm)
        ga2 = [sb(f"ga2{i}", [128, D], F32, sm) for i in range(NB)]
        for i in range(NB):
            P.dma("sp", ga2[i][:], mod_d[i:i + 1, 5 * D:6 * D].partition_broadcast(128), reads=["mod_d"], writes=[("ga2", i)])
        A = affT_all[0:NP, :]
        for r in range(CAP // 8):
            rs = slice(r * 8, (r + 1) * 8)
            P.op("dve", lambda e, rs=rs: e.max(out=vals[:, rs], in_=A), reads=["affT_all"], writes=["vals"])
            P.op("dve", lambda e, rs=rs: e.max_index(out=idxu[:, rs], in_max=vals[:, rs], in_values=A),
                 reads=["affT_all", "vals"], writes=["idxu"])
            P.op("dve", lambda e, rs=rs: e.match_replace(out=A, in_to_replace=vals[:, rs], in_values=A, imm_value=-1.0),
                 reads=["vals"], writes=["affT_all"])
        P.op("dve", lambda e: e.tensor_copy(out=idxf[:], in_=idxu[:]), reads=["idxu"], writes=["idxf"])
        for hh in range(2):
            P.op("pe", lambda e, hh=hh: e.transpose(out=PSB[0][:, hh * 64:hh * 64 + NP], in_=idxf[0:NP, hh * 128:(hh + 1) * 128],
                                                     identity=ident32[0:NP, 0:NP]), reads=["idxf", "ident32"], writes=[psk(0)])
            P.op("pe", lambda e, hh=hh: e.transpose(out=PSB[1][:, hh * 64:hh * 64 + NP], in_=vals[0:NP, hh * 128:(hh + 1) * 128],
                                                     identity=ident32[0:NP, 0:NP]), reads=["vals", "ident32"], writes=[psk(1)])
        for hh in range(2):
            P.op("dve", lambda e, hh=hh: e.tensor_copy(out=idxT[:, hh, 0:NP], in_=PSB[0][:, hh * 64:hh * 64 + NP]), reads=[psk(0)], writes=["idxT"])
            P.op("dve", lambda e, hh=hh: e.tensor_copy(out=valsT[:, hh, 0:NP], in_=PSB[1][:, hh * 64:hh * 64 + NP]), reads=[psk(1)], writes=["valsT"])
        dbg_out("idx", idxu[:], ["idxu"])
        dbg_out("vals", vals[:], ["vals"])
        sx = ExitStack()
        wg = [sb(f"wg{i}", [128, 8, D], BF16, sx) for i in range(2)]
        wu = [sb(f"wu{i}", [128, 8, D], BF16, sx) for i in range(2)]
        wd = [sb(f"wd{i}", [128, 8, D], BF16, sx) for i in range(2)]
        XT2 = [sb(f"XT{i}", [128, 8, NS], BF16, sx) for i in range(2)]
        hidT = sb("hidT", [128, 8, NS], BF16, sx)
        xg = [sb(f"xg{i}", [128, D], BF16, sx) for i in range(2)]
        yt = [sb(f"yt{i}", [128, D], F32, sx) for i in range(2)]
        nn = min(512, NS)
        nblk = NS // nn
        sgt = [sb("sgt0", [128, nn], F32, sx)] * 2

        while not prepass_done():
            prepass_step()

        def load_expert(e_):
            i = e_ % 2
            for m, (wt, nm, q_) in enumerate(((wg, "wg", "sp"), (wu, "wu", "act"), (wd, "wd", "sp"))):
                P.dma(q_, wt[i][:], webf_d[m][e_].rearrange("(kc p) n -> p kc n", p=128), reads=[("webf", m, e_)], writes=[(nm, i)])
        cntg = [0]

        def gather_step(e_, st_):
            b, hh = st_ // 2, st_ % 2
            XT = XT2[e_ % 2]
            kx = ("XT", e_ % 2)
            i = cntg[0] % 2
            cntg[0] += 1
            col = b * NE + e_
            P.dma("pool", None, None, reads=[("x1n_d", b), "idxT"], writes=[("xg", i)],
                  fn=lambda g, i=i, b=b, hh=hh, col=col: g.indirect_dma_start(
                      out=xg[i][:, :], out_offset=None, in_=x1n_d[b][:, :],
                      in_offset=bass.IndirectOffsetOnAxis(ap=idxT[:, hh, col:col + 1], axis=0)))
            pT = PSB[i][:].bitcast(BF16)
            for kc in range(8):
                P.op("pe", lambda e, kc=kc, i=i, pT=pT: e.transpose(out=pT[:, kc * 128:(kc + 1) * 128], in_=xg[i][:, kc * 128:(kc + 1) * 128],
                                                                     identity=identbf[:]),
                     reads=[("xg", i), "identbf"], writes=[psk(i)], inc=(kc == 7))
            c0 = st_ * 128
            for kc in range(8):
                src = pT[:, kc * 128:(kc + 1) * 128]
                if i == 0:
                    P.op("act", lambda e, kc=kc, src=src, c0=c0, b=b, XT=XT: e.activation(out=XT[:, kc, c0:c0 + 128], in_=src, func=AF.Identity,
                                                                                          scale=scale2T[:, kc, b:b + 1], bias=modT[:, 24 + kc, b:b + 1]),
                         reads=[psk(i), "scale2T", "modT"], writes=[(kx, kc)])
                else:
                    P.op("dve", lambda e, kc=kc, src=src, c0=c0, b=b, XT=XT: e.tensor_scalar(out=XT[:, kc, c0:c0 + 128], in0=src,
                                                                                             scalar1=scale2T[:, kc, b:b + 1],
                                                                                             scalar2=modT[:, 24 + kc, b:b + 1], op0=ALU.mult, op1=ALU.add),
                         reads=[psk(i), "scale2T", "modT"], writes=[(kx, kc)])

        NST = NB * 2
        for st_ in range(NST):
            gather_step(0, st_)
        load_expert(0)
        for e_ in range(NE):
            if e_ + 1 < NE:
                load_expert(e_ + 1)
            wi = e_ % 2
            XT = XT2[e_ % 2]
            kxt = ("XT", e_ % 2)
            nxt = list(range(NST)) if e_ + 1 < NE else []
            for fc in range(8):
                fsl = slice(fc * 128, (fc + 1) * 128)
                for blk in range(nblk):
                    bsl_ = slice(blk * nn, (blk + 1) * nn)
                    j = (fc * nblk + blk) % 2
                    bg_, bu_ = 2 + j * 2, 3 + j * 2
                    for (wt, nm, bk) in ((wg, "wg", bg_), (wu, "wu", bu_)):
                        for kc in range(8):
                            P.op("pe", lambda e, kc=kc, wt=wt, bk=bk, fsl=fsl, bsl_=bsl_: e.matmul(
                                PSB[bk][:, 0:nn], lhsT=wt[wi][:, kc, fsl], rhs=XT[:, kc, bsl_], start=(kc == 0), stop=(kc == 7)),
                                 reads=[(nm, wi), (kxt, kc)], writes=[psk(bk)], inc=(kc == 7))
                    P.op("act", lambda e, j=j, bg_=bg_: e.activation(out=sgt[j][:], in_=PSB[bg_][:, 0:nn], func=AF.Silu),
                         reads=[psk(bg_)], writes=[("sgt", 0)])
                    P.op("dve", lambda e, j=j, bu_=bu_, fc=fc, bsl_=bsl_: e.tensor_tensor(out=hidT[:, fc, bsl_], in0=PSB[bu_][:, 0:nn], in1=sgt[j][:],
                                                                                          op=ALU.mult),
                         reads=[psk(bu_), ("sgt", 0)], writes=["hidT"])
                per = -(-NST // 8)
                for _ in range(per):
                    if nxt:
                        gather_step(e_ + 1, nxt.pop(0))
            for st_ in range(NB * 2):
                b, hh = st_ // 2, st_ % 2
                col = b * NE + e_
                i = st_ % 2
                for half in range(2):
                    hs_ = slice(half * 512, (half + 1) * 512)
                    bk = (6 + half) if st_ % 2 == 0 else half
                    for fc in range(8):
                        P.op("pe", lambda e, fc=fc, st_=st_, hs_=hs_, bk=bk: e.matmul(PSB[bk][:, :], lhsT=hidT[:, fc, st_ * 128:(st_ + 1) * 128],
                                                                                       rhs=wd[wi][:, fc, hs_], start=(fc == 0), stop=(fc == 7)),
                             reads=["hidT", ("wd", wi)], writes=[psk(bk)], inc=(fc == 7))
                    P.op("dve", lambda e, i=i, hs_=hs_, bk=bk, hh=hh, col=col, b=b: e.scalar_tensor_tensor(
                        out=yt[i][:, hs_], in0=PSB[bk][:, :], scalar=valsT[:, hh, col:col + 1], in1=ga2[b][:, hs_], op0=ALU.mult, op1=ALU.mult),
                         reads=[psk(bk), "valsT", ("ga2", b)], writes=[("yt", i)])
                P.dma("pool", None, None, reads=[("yt", i), "idxT"], writes=[("x1_d", b)],
                      fn=lambda g, i=i, b=b, hh=hh, col=col: g.indirect_dma_start(
                          out=x1_d[b][:, :], out_offset=bass.IndirectOffsetOnAxis(ap=idxT[:, hh, col:col + 1], axis=0),
                          in_=yt[i][:, :], in_offset=None, compute_op=ALU.add))
        P.barrier()
        sx.close()
        gfin = sb("gfin", [128, D], F32, sm)
        P.dma("sp", gfin[:], gvec_d[2:3, :].partition_broadcast(128), writes=["gfin"])
        NF = 4
        xf = [sb(f"xf{i}", [128, D], F32, sm) for i in range(NF)]
        of = [sb(f"of{i}", [128, D], F32, sm) for i in range(NF)]
        junkf = sb("junkf", [128, D], BF16, sm)
        smf = [sb(f"smf{i}", [128, 4], F32, sm) for i in range(NF)]
        cf = 0
        for b in range(NB):
            for t in range(NT):
                i = cf % NF
                cf += 1
                rows = slice(t * 128, (t + 1) * 128)
                P.dma("sp", xf[i][:], x1_d[b][rows, :], reads=[("x1_d", b)], writes=[("xf", i)])
                rms_rstd("dve", xf[i][:], junkf[:], smf[i][:, 0:1], smf[i][:, 1:2], smf[i][:, 2:3], ("xf", i), f"ssf{i}")
                P.op("dve", lambda e, i=i: e.scalar_tensor_tensor(out=of[i][:], in0=xf[i][:], scalar=smf[i][:, 2:3], in1=gfin[:],
                                                                  op0=ALU.mult, op1=ALU.mult),
                     reads=[("xf", i), f"ssf{i}rstd", "gfin"], writes=[("of", i)])
                P.dma("pool", out_d[b, rows, :], of[i][:], reads=[("of", i)], writes=[("out", b, t)])
        P.barrier()
    es.close()
    return nc


_NC_CACHE = {}


def _core_inputs(inp, bs, consts):
    m = dict(consts)
    m["x"] = np.ascontiguousarray(inp["x"][bs], dtype=np.float32)
    m["ctx"] = np.ascontiguousarray(inp["ctx"][bs], dtype=np.float32)
    cv = np.zeros((5, D), np.float32)
    cc = np.asarray(inp["c"][bs], dtype=np.float32)
    cv[:cc.shape[0]] = cc
    cv[4] = np.asarray(inp["c_ctx"], dtype=np.float32)
    m["cvec"] = cv
    m["w_ada"] = np.ascontiguousarray(inp["w_ada"][0])
    m["b_ada"] = np.ascontiguousarray(inp["b_ada"]).reshape(1, 6 * D)
    m["gvec"] = np.ascontiguousarray(np.stack([inp["g_norm1"][0], inp["g_norm2"][0], inp["g_final"]]))
    m["w_in"] = np.ascontiguousarray(inp["w_in"][0])
    m["b_gates"] = np.ascontiguousarray(inp["b_gates"]).reshape(1, 16)
    m["w_conv"] = np.ascontiguousarray(inp["w_conv"][0]).reshape(9, D)
    m["w_fourier"] = np.ascontiguousarray(inp["w_fourier"][0])
    m["w_mlstm"] = np.ascontiguousarray(inp["w_mlstm"][0])
    m["w_out"] = np.ascontiguousarray(inp["w_out"][0])
    m["w_router"] = np.ascontiguousarray(inp["w_router"][0])
    m["w_gate_e"] = np.ascontiguousarray(inp["w_gate_e"][0])
    m["w_up_e"] = np.ascontiguousarray(inp["w_up_e"][0])
    m["w_down_e"] = np.ascontiguousarray(inp["w_down_e"][0])
    return m


def kernel(**inputs):
    inp = {k: np.asarray(v) for k, v in inputs.items()}
    B = inp["x"].shape[0]
    nb = B // NCORES
    if "c" not in _CONST_CACHE:
        _CONST_CACHE["c"] = _consts()
    consts = _CONST_CACHE["c"]
    if nb not in _NC_CACHE:
        _NC_CACHE[nb] = build(nb)
    nc = _NC_CACHE[nb]
    in_maps = [_core_inputs(inp, slice(i * nb, (i + 1) * nb), consts) for i in range(NCORES)]
    res = run_bass_kernel_spmd(nc, in_maps, core_ids=list(range(NCORES)))
    out = np.concatenate([np.asarray(r["out"]) for r in res.results], axis=0)
    return out.astype(np.float32, copy=False)
```

```python
import numpy as np
import ml_dtypes
from contextlib import ExitStack
import concourse.bass as bass
import concourse.mybir as mybir
from concourse.bass_utils import run_bass_kernel_spmd

F32 = mybir.dt.float32
BF16 = mybir.dt.bfloat16
I32 = mybir.dt.int32
U32 = mybir.dt.uint32
AF = mybir.ActivationFunctionType
ALU = mybir.AluOpType

D = 1024
L = 2048
NT = L // 128
CL = 256
NE = 16
CAP = 256
DH = 128
OFF_F, OFF_QK, OFF_V, OFF_O, OFF_G, OFF_BR, IN_COLS = 0, 512, 1536, 2048, 2560, 2576, 4624
EPS = 1e-6
NCORES = 8
NBFULL = 4
SAME_ENGINE_NOWAIT = ("pe",)


class Prog:
    ENG = ("pe", "act", "dve", "pool", "sp")
    LIMIT = 28000

    def __init__(self, nc, es):
        self.nc = nc
        self.es = es
        self.h = {"pe": nc.tensor, "act": nc.scalar, "dve": nc.vector, "pool": nc.gpsimd, "sp": nc.sync}
        self.count = {e: 0 for e in self.ENG}
        self.epoch = {e: 0 for e in self.ENG}
        self.last_tok = {e: None for e in self.ENG}
        self.sem = {}
        self.seen = {e: {} for e in self.ENG}
        self.lastw = {}
        self.readers = {}
        self.rings = {}
        self.nsem = 0
        self.bank_last = {}
        for e in self.ENG:
            self._newsem(e, 0)
        for q, n in (("sp", 20), ("pool", 10), ("act", 6)):
            names = []
            for j in range(n):
                nm = f"{q}_d{j}"
                self.sem[(nm, 0)] = es.enter_context(nc.semaphore(nm))
                names.append(nm)
            self.rings[q] = dict(names=names, vals=[0] * n, nxt=0)

    def _newsem(self, e, ep):
        self.sem[(e, ep)] = self.es.enter_context(self.nc.semaphore(f"s_{e}_{ep}"))

    def _wait(self, e, tok):
        src, ep, val = tok
        if src == e and e in SAME_ENGINE_NOWAIT:
            return
        cur = self.seen[e].get(src, (-1, 0))
        if (ep, val) <= cur:
            return
        self.h[e].wait_ge(self.sem[(src, ep)], val)
        self.seen[e][src] = (ep, val)

    def _deps(self, e, reads, writes):
        toks = set()
        for k in reads:
            t = self.lastw.get(k)
            if t is not None:
                toks.add(t)
        for k in writes:
            t = self.lastw.get(k)
            if t is not None:
                toks.add(t)
            for t in self.readers.get(k, {}).values():
                toks.add(t)
        for t in sorted(toks):
            self._wait(e, t)

    def _record(self, tok, reads, writes):
        for k in reads:
            self.readers.setdefault(k, {})[tok[0]] = tok
        for k in writes:
            self.lastw[k] = tok
            self.readers[k] = {}

    @staticmethod
    def _bank_of(k):
        if isinstance(k, tuple) and len(k) >= 2:
            if k[0] == "psb":
                return k[1]
            if k[0] == "ps0":
                return 0
            if k[0] == "ps1":
                return 1
            if k[0] == "pO":
                return 2 + (k[1] % 2)
        return None

    def op(self, e, fn, reads=(), writes=(), inc=True):
        assert inc or e == "pe"
        self._deps(e, reads, writes)
        banks = {self._bank_of(k) for k in tuple(reads) + tuple(writes)} - {None}
        for bk in sorted(banks):
            for oe, t in sorted(self.bank_last.get(bk, {}).items()):
                if oe != e:
                    self._wait(e, t)
        ins = fn(self.h[e])
        if inc:
            self.count[e] += 1
            ins.then_inc(self.sem[(e, self.epoch[e])], 1)
            tok = (e, self.epoch[e], self.count[e])
            self.last_tok[e] = tok
            if self.count[e] >= self.LIMIT:
                self.epoch[e] += 1
                self.count[e] = 0
                self._newsem(e, self.epoch[e])
        else:
            tok = (e, self.epoch[e], self.count[e] + 1)
        for bk in banks:
            self.bank_last.setdefault(bk, {})[e] = tok
        self._record(tok, reads, writes)
        return tok

    def dma(self, q, out, in_, reads=(), writes=(), fn=None):
        ring = self.rings[q]
        j = ring["nxt"]
        ring["nxt"] = (j + 1) % len(ring["names"])
        nm = ring["names"][j]
        if ring["vals"][j] > 0:
            self._wait(q, (nm, 0, ring["vals"][j]))
        self._deps(q, reads, writes)
        if fn is None:
            ins = self.h[q].dma_start(out=out, in_=in_)
        else:
            ins = fn(self.h[q])
        ins.then_inc(self.sem[(nm, 0)], 16)
        ring["vals"][j] += 16
        tok = (nm, 0, ring["vals"][j])
        self._record(tok, reads, writes)
        return tok

    def barrier(self):
        toks = [t for t in self.last_tok.values() if t is not None]
        for ring in self.rings.values():
            for nm, v in zip(ring["names"], ring["vals"]):
                if v > 0:
                    toks.append((nm, 0, v))
        for e in self.ENG:
            for t in sorted(toks):
                self._wait(e, t)


def _consts():
    bf = ml_dtypes.bfloat16
    c = {}
    c["ident32"] = np.eye(128, dtype=np.float32)
    c["identbf"] = np.eye(128, dtype=np.float32).astype(bf)
    s = np.arange(128)
    same = np.ones((128, 128), dtype=bool)
    c["maskF"] = (same & (s[:, None] <= s[None, :])).astype(np.float32) * DH ** -0.5
    c["maskB"] = (same & (s[:, None] >= s[None, :])).astype(np.float32) * DH ** -0.5
    c["triincl"] = (same & (s[:, None] <= s[None, :])).astype(np.float32)
    c["blockones"] = same.astype(np.float32)
    c["sel0"] = np.repeat((s < 64).astype(np.float32)[:, None], 128, 1)
    c["sel1"] = np.repeat((s >= 64).astype(np.float32)[:, None], 128, 1)
    a = 2 * np.pi * np.outer(s, s) / 128.0
    c["ccs"] = np.concatenate([np.cos(a), -np.sin(a)], 1).astype(np.float32) * 128 ** -0.5
    c["ccs"] = c["ccs"].astype(bf)
    l = np.arange(L // 2, dtype=np.int64)
    sc = L ** -0.5
    tabC, tabS = [], []
    for e_ in range(2):
        for j in range(2):
            lp = 2 * (j * 512 + np.arange(512, dtype=np.int64)) + e_
            ang = 2 * np.pi * ((l[:, None] * lp[None, :]) % L).astype(np.float64) / L
            for tab, m in ((tabC, np.cos(ang) * sc), (tabS, np.sin(ang) * sc)):
                m = m.astype(np.float32).reshape(8, 128, 512)
                tab.append(np.ascontiguousarray(m.transpose(1, 0, 2)))
    c["dftC"] = np.stack(tabC).astype(bf)
    c["dftS"] = np.stack(tabS).astype(bf)
    return c


_CONST_CACHE = {}


def build(NB, dbg=(), skip_moe=False):
    nc = bass.Bass("TRN2", target_bir_lowering=False)
    es = ExitStack()
    P = Prog(nc, es)

    def din(name, shape, dt=F32):
        return nc.dram_tensor(name, list(shape), dt, kind="ExternalInput").ap()

    def dscr(name, shape, dt):
        return nc.dram_tensor(name, list(shape), dt, kind="Internal").ap()

    x_d = din("x", [NB, L, D])
    ctx_d = din("ctx", [NB, CL, D])
    cvec_d = din("cvec", [5, D])
    wada_d = din("w_ada", [D, 6 * D])
    bada_d = din("b_ada", [1, 6 * D])
    gvec_d = din("gvec", [3, D])
    win_d = din("w_in", [D, IN_COLS])
    bg_d = din("b_gates", [1, 16])
    wconv_d = din("w_conv", [9, D])
    wf_d = din("w_fourier", [512, D])
    wm_d = din("w_mlstm", [512, D])
    wo_d = din("w_out", [D, D])
    wr_d = din("w_router", [D, NE])
    wge_d = din("w_gate_e", [NE, D, D])
    wue_d = din("w_up_e", [NE, D, D])
    wde_d = din("w_down_e", [NE, D, D])
    cd = {}
    for nm, shp, dt in (("ident32", [128, 128], F32), ("identbf", [128, 128], BF16), ("maskF", [128, 128], F32),
                        ("maskB", [128, 128], F32), ("triincl", [128, 128], F32), ("blockones", [128, 128], F32),
                        ("sel0", [128, 128], F32), ("sel1", [128, 128], F32), ("ccs", [128, 256], BF16),
                        ("dftC", [4, 128, 8, 512], BF16), ("dftS", [4, 128, 8, 512], BF16)):
        cd[nm] = din(nm, shp, dt)
    out_d = nc.dram_tensor("out", [NB, L, D], F32, kind="ExternalOutput").ap()
    dbg_d = {}
    for nm, shp, dt in dbg:
        dbg_d[nm] = nc.dram_tensor("dbg_" + nm, list(shp), dt, kind="ExternalOutput").ap()

    mod_d = dscr("mod_d", [5, 6 * D], F32)
    winbf_d = dscr("winbf_d", [D, IN_COLS], BF16)
    wfold_d = dscr("wfold_d", [D, D], BF16)
    wmbf_d = dscr("wmbf_d", [512, D], BF16)
    woutbf_d = dscr("woutbf_d", [D, D], BF16)
    pq_d = dscr("pq_d", [8, 128, L], BF16)
    hf_d = dscr("hf_d", [L, 512], BF16)
    webf_d = [dscr(f"webf{m}", [NE, D, D], BF16) for m in range(3)]
    x1_d = [dscr(f"x1_d{i}", [L, D], F32) for i in range(NB)]
    x1n_d = [dscr(f"x1n_d{i}", [L, D], BF16) for i in range(NB)]

    uniq = [0]

    def sb(name, shape, dt, stack=None):
        uniq[0] += 1
        return (stack or es).enter_context(nc.sbuf_tensor(f"s{uniq[0]}_{name}", list(shape), dt))

    def ps(name, shape, dt, stack=None):
        uniq[0] += 1
        return (stack or es).enter_context(nc.psum_tensor(f"p{uniq[0]}_{name}", list(shape), dt))

    PSB = [ps(f"psb{i}", [128, 512], F32) for i in range(8)]

    def psk(i):
        return ("psb", i)

    ident32 = sb("ident32", [128, 128], F32)
    identbf = sb("identbf", [128, 128], BF16)
    maskF = sb("maskF", [128, 128], F32)
    maskB = sb("maskB", [128, 128], F32)
    triincl = sb("triincl", [128, 128], F32)
    blockones = sb("blockones", [128, 128], F32)
    sel0 = sb("sel0", [128, 128], F32)
    sel1 = sb("sel1", [128, 128], F32)
    ccs = sb("ccs", [128, 256], BF16)
    modT = sb("modT", [128, 48, 5], F32)
    gT = sb("gT", [128, 8, 3], F32)
    wcT = sb("wcT", [128, 8, 9], F32)
    scale1T = sb("scale1T", [128, 8, 5], F32)
    scale2T = sb("scale2T", [128, 8, 5], F32)
    wr_sb = sb("wr_sb", [128, 8, NE], F32)
    bg_sb = sb("bg_sb", [128, 16], F32)
    eps_sb = sb("eps_sb", [128, 1], F32)
    S32 = sb("S32", [128, 8, 130], F32)
    Sbf = sb("Sbf", [128, 8, 130], BF16)
    affT_all = sb("affT_all", [64, L], F32)
    wst = [sb(f"wst{i}", [128, 2, D], BF16) for i in range(2)]
    pre_src = (wge_d, wue_d, wde_d)
    pre_steps = [(e_, m, q) for e_ in range(NE) for m in range(3) for q in range(4)]
    pre_state = dict(k=0, pending=None)

    def prepass_step():
        k = pre_state["k"]
        if k < len(pre_steps):
            e_, m, q = pre_steps[k]
            i = k % 2
            P.dma("pool", wst[i][:], pre_src[m][e_, q * 256:(q + 1) * 256, :].rearrange("(k p) n -> p k n", p=128),
                  writes=[("wst", i)])
        if pre_state["pending"] is not None:
            e2, m2, q2, i2 = pre_state["pending"]
            P.dma("pool", webf_d[m2][e2, q2 * 256:(q2 + 1) * 256, :].rearrange("(k p) n -> p k n", p=128), wst[i2][:],
                  reads=[("wst", i2)], writes=[("webf", m2, e2)])
            pre_state["pending"] = None
        if k < len(pre_steps):
            pre_state["pending"] = (e_, m, q, i)
            pre_state["k"] = k + 1

    def prepass_done():
        return pre_state["k"] >= len(pre_steps) and pre_state["pending"] is None

    for nm, t in (("ident32", ident32), ("identbf", identbf), ("maskF", maskF), ("maskB", maskB),
                  ("triincl", triincl), ("blockones", blockones), ("sel0", sel0), ("sel1", sel1), ("ccs", ccs)):
        P.dma("sp", t[:], cd[nm], writes=[nm])
    P.dma("sp", wr_sb[:], wr_d.rearrange("(kc p) e -> p kc e", p=128), writes=["wr"])
    P.dma("sp", bg_sb[:], bg_d.partition_broadcast(128), writes=["bg"])
    P.op("dve", lambda e: e.memset(eps_sb[:], EPS), writes=["eps"])
    mhalf_sb = sb("mhalf_sb", [128, 1], F32)
    P.op("dve", lambda e: e.memset(mhalf_sb[:], -0.5), writes=["mhalf"])
    P.op("pool", lambda e: e.memset(affT_all[:], 0.0), writes=["affT_all"])

    def dbg_out(nm, src_ap, reads, dst=None):
        if nm in dbg_d:
            P.dma("sp", dst if dst is not None else dbg_d[nm], src_ap, reads=reads, writes=[("dbg", nm)])

    with ExitStack() as st:
        cv = sb("cv", [5, D], F32, st)
        csil = sb("csil", [5, D], F32, st)
        cT = sb("cT", [128, 8, 5], F32, st)
        modsb = sb("modsb", [5, 6 * D], F32, st)
        badasb = sb("badasb", [5, 6 * D], F32, st)
        wa = [sb(f"wa{i}", [128, 8, 512], F32, st) for i in range(2)]
        gv = sb("gv", [3, D], F32, st)
        wcv = sb("wcv", [9, D], F32, st)
        P.dma("sp", cv[:], cvec_d, writes=["cv"])
        P.dma("sp", badasb[:], bada_d.partition_broadcast(5), writes=["bada"])
        P.dma("sp", gv[:], gvec_d, writes=["gv"])
        P.dma("sp", wcv[:], wconv_d, writes=["wcv"])
        P.op("act", lambda e: e.activation(out=csil[:], in_=cv[:], func=AF.Silu), reads=["cv"], writes=["csil"])
        pA = PSB[0]
        for kc in range(8):
            P.op("pe", lambda e, kc=kc: e.transpose(out=pA[:, kc * 5:(kc + 1) * 5], in_=csil[0:5, kc * 128:(kc + 1) * 128],
                                                     identity=ident32[0:5, 0:5]),
                 reads=["csil", "ident32"], writes=[psk(0)], inc=(kc == 7))
        P.op("dve", lambda e: e.tensor_copy(out=cT[:].rearrange("p a b -> p (a b)"), in_=pA[:, 0:40]),
             reads=[psk(0)], writes=["cT"])
        for cb in range(12):
            w = wa[cb % 2]
            P.dma("sp" if cb % 2 == 0 else "act", w[:],
                  wada_d[:, cb * 512:(cb + 1) * 512].rearrange("(kc p) n -> p kc n", p=128), writes=[("wa", cb % 2)])
            pb = PSB[1 + cb % 2]
            for kc in range(8):
                P.op("pe", lambda e, kc=kc, w=w, pb=pb: e.matmul(pb[0:5, :], lhsT=cT[:, kc, :], rhs=w[:, kc, :],
                                                                   start=(kc == 0), stop=(kc == 7)),
                     reads=["cT", ("wa", cb % 2)], writes=[psk(1 + cb % 2)], inc=(kc == 7))
            P.op("dve", lambda e, cb=cb, pb=pb: e.tensor_tensor(out=modsb[:, cb * 512:(cb + 1) * 512], in0=pb[0:5, :],
                                                                in1=badasb[:, cb * 512:(cb + 1) * 512], op=ALU.add),
                 reads=[psk(1 + cb % 2), "bada"], writes=["modsb"])
        P.dma("sp", mod_d, modsb[:], reads=["modsb"], writes=["mod_d"])
        dbg_out("mod", modsb[:], ["modsb"])
        pA = PSB[3]
        for j in range(48):
            P.op("pe", lambda e, j=j: e.transpose(out=pA[:, j * 5:(j + 1) * 5], in_=modsb[0:5, j * 128:(j + 1) * 128],
                                                   identity=ident32[0:5, 0:5]),
                 reads=["modsb", "ident32"], writes=[psk(3)], inc=(j == 47))
        P.op("dve", lambda e: e.tensor_copy(out=modT[:].rearrange("p a b -> p (a b)"), in_=pA[:, 0:240]),
             reads=[psk(3)], writes=["modT"])
        pA = PSB[4]
        for kc in range(8):
            P.op("pe", lambda e, kc=kc: e.transpose(out=pA[:, kc * 3:(kc + 1) * 3], in_=gv[0:3, kc * 128:(kc + 1) * 128],
                                                     identity=ident32[0:3, 0:3]),
                 reads=["gv", "ident32"], writes=[psk(4)], inc=(kc == 7))
        P.op("dve", lambda e: e.tensor_copy(out=gT[:].rearrange("p a b -> p (a b)"), in_=pA[:, 0:24]),
             reads=[psk(4)], writes=["gT"])
        pA = PSB[5]
        for kc in range(8):
            P.op("pe", lambda e, kc=kc: e.transpose(out=pA[:, kc * 9:(kc + 1) * 9], in_=wcv[0:9, kc * 128:(kc + 1) * 128],
                                                     identity=ident32[0:9, 0:9]),
                 reads=["wcv", "ident32"], writes=[psk(5)], inc=(kc == 7))
        P.op("dve", lambda e: e.tensor_copy(out=wcT[:].rearrange("p a b -> p (a b)"), in_=pA[:, 0:72]),
             reads=[psk(5)], writes=["wcT"])
        for kc in range(8):
            P.op("dve", lambda e, kc=kc: e.tensor_scalar(out=scale1T[:, kc, :], in0=modT[:, 8 + kc, :], scalar1=1.0,
                                                          scalar2=gT[:, kc, 0:1], op0=ALU.add, op1=ALU.mult),
                 reads=["modT", "gT"], writes=["scale1T"])
            P.op("dve", lambda e, kc=kc: e.tensor_scalar(out=scale2T[:, kc, :], in0=modT[:, 32 + kc, :], scalar1=1.0,
                                                          scalar2=gT[:, kc, 1:2], op0=ALU.add, op1=ALU.mult),
                 reads=["modT", "gT"], writes=["scale2T"])
        wtmp = [sb(f"wtmp{i}", [128, 8, 512], BF16, st) for i in range(2)]
        ngr = (IN_COLS + 511) // 512
        for g in range(ngr):
            c0 = g * 512
            w_ = min(512, IN_COLS - c0)
            t = wtmp[g % 2]
            P.dma("pool", t[:, :, 0:w_], win_d[:, c0:c0 + w_].rearrange("(kc p) n -> p kc n", p=128),
                  writes=[("wtmp", g % 2)])
            P.dma("sp", winbf_d[:, c0:c0 + w_].rearrange("(kc p) n -> p kc n", p=128), t[:, :, 0:w_],
                  reads=[("wtmp", g % 2)], writes=["winbf_d"])
        wfbf = sb("wfbf", [128, 4, D], BF16, st)
        wfo = sb("wfo", [128, 8, D], BF16, st)
        P.dma("pool", wfbf[:], wf_d.rearrange("(g p) n -> p g n", p=128), writes=["wfbf"])
        i = 0
        for g in range(4):
            for m in range(2):
                for hf in range(2):
                    bk = 6 + (i % 2)
                    pb = PSB[bk]
                    P.op("pe", lambda e, g=g, m=m, hf=hf, pb=pb: e.matmul(pb[:, :], lhsT=ccs[:, m * 128:(m + 1) * 128],
                                                                           rhs=wfbf[:, g, hf * 512:(hf + 1) * 512],
                                                                           start=True, stop=True),
                         reads=["ccs", "wfbf"], writes=[psk(bk)])
                    P.op("act" if i % 2 == 0 else "dve",
                         (lambda e, g=g, m=m, hf=hf, pb=pb: e.activation(out=wfo[:, m * 4 + g, hf * 512:(hf + 1) * 512],
                                                                         in_=pb[:, :], func=AF.Copy)) if i % 2 == 0 else
                         (lambda e, g=g, m=m, hf=hf, pb=pb: e.tensor_copy(out=wfo[:, m * 4 + g, hf * 512:(hf + 1) * 512],
                                                                          in_=pb[:, :])),
                         reads=[psk(bk)], writes=["wfo"])
                    i += 1
        P.dma("sp", wfold_d.rearrange("(kc p) n -> p kc n", p=128), wfo[:], reads=["wfo"], writes=["wfold_d"])
        wcast = sb("wcast", [128, 12, D], BF16, st)
        P.dma("pool", wcast[:, 0:8, :], wo_d.rearrange("(kc p) n -> p kc n", p=128), writes=["wcast_o"])
        P.dma("pool", wcast[:, 8:12, :], wm_d.rearrange("(kc p) n -> p kc n", p=128), writes=["wcast_m"])
        P.dma("sp", woutbf_d.rearrange("(kc p) n -> p kc n", p=128), wcast[:, 0:8, :], reads=["wcast_o"], writes=["woutbf_d"])
        P.dma("sp", wmbf_d.rearrange("(kc p) n -> p kc n", p=128), wcast[:, 8:12, :], reads=["wcast_m"], writes=["wmbf_d"])
        P.barrier()

    bst = ExitStack()
    hmT = sb("hmT", [128, 4, L], BF16, bst)
    gates = gsm = dec = None

    def rms_rstd(eng_sq, xt, junk, ss, rt, rstd, kx, kss):
        P.op("dve", lambda e: e.scalar_tensor_tensor(out=junk, in0=xt, scalar=1.0, in1=xt, op0=ALU.mult, op1=ALU.mult,
                                                     accum_out=ss), reads=[kx], writes=[kss + "junk", kss])
        P.op("act", lambda e: e.activation(out=rt, in_=ss, func=AF.Sqrt, scale=1.0 / D, bias=eps_sb[:, 0:1]),
             reads=[kss, "eps"], writes=[kss + "rt"])
        P.op("dve", lambda e: e.reciprocal(out=rstd, in_=rt), reads=[kss + "rt"], writes=[kss + "rstd"])

    def norm_to_T(src_tile_ap, ntiles, r, dstT, kdst, st, tag):
        xt = [sb(f"xt{tag}{i}", [128, D], F32, st) for i in range(3)]
        junk = sb(f"junk{tag}", [128, D], BF16, st)
        xn = [sb(f"xn{tag}{i}", [128, D], BF16, st) for i in range(3)]
        sm = [sb(f"sm{tag}{i}", [128, 4], F32, st) for i in range(3)]
        psTs = [PSB[j][:].bitcast(BF16) for j in range(4)]
        for t in range(ntiles):
            i = t % 3
            ib = t % 2
            P.dma("sp", xt[i][:], src_tile_ap(t), writes=[("xt", tag, i)])
            rms_rstd("dve", xt[i][:], junk[:], sm[i][:, 0:1], sm[i][:, 1:2], sm[i][:, 2:3], ("xt", tag, i), f"ss{tag}{i}")
            P.op("act", lambda e, i=i: e.activation(out=xn[i][:], in_=xt[i][:], func=AF.Copy, scale=sm[i][:, 2:3]),
                 reads=[("xt", tag, i), f"ss{tag}{i}rstd"], writes=[("xn", tag, i)])
            for kc in range(8):
                bk = 2 * ib + kc // 4
                pT = psTs[bk]
                P.op("pe", lambda e, kc=kc, i=i, pT=pT: e.transpose(out=pT[:, (kc % 4) * 128:(kc % 4 + 1) * 128],
                                                                     in_=xn[i][:, kc * 128:(kc + 1) * 128],
                                                                     identity=identbf[:]),
                     reads=[("xn", tag, i), "identbf"], writes=[psk(bk)], inc=(kc % 4 == 3))
            for kc in range(8):
                bk = 2 * ib + kc // 4
                pT = psTs[bk]
                src = pT[:, (kc % 4) * 128:(kc % 4 + 1) * 128]
                if kc // 4 == 0:
                    P.op("act", lambda e, kc=kc, t=t, src=src: e.activation(out=dstT[:, kc, t * 128:(t + 1) * 128], in_=src,
                                                                            func=AF.Identity, scale=scale1T[:, kc, r:r + 1],
                                                                            bias=modT[:, kc, r:r + 1]),
                         reads=[psk(bk), "scale1T", "modT"], writes=[(kdst, kc)])
                else:
                    P.op("dve", lambda e, kc=kc, t=t, src=src: e.tensor_scalar(out=dstT[:, kc, t * 128:(t + 1) * 128], in0=src,
                                                                               scalar1=scale1T[:, kc, r:r + 1],
                                                                               scalar2=modT[:, kc, r:r + 1],
                                                                               op0=ALU.mult, op1=ALU.add),
                         reads=[psk(bk), "scale1T", "modT"], writes=[(kdst, kc)])

    def load_wcols(tile, c0, w_, key, q="sp"):
        P.dma(q, tile[:, :, 0:w_], winbf_d[:, c0:c0 + w_].rearrange("(kc p) n -> p kc n", p=128),
              reads=["winbf_d"], writes=[key])

    def proj_tok(hT, ntiles, wt, wkey, ncols, evac, banks, hkey="hT"):
        for t in range(ntiles):
            bk = banks[t % len(banks)]
            pb = PSB[bk]
            for kc in range(8):
                P.op("pe", lambda e, kc=kc, t=t, pb=pb: e.matmul(pb[:, 0:ncols], lhsT=hT[:, kc, t * 128:(t + 1) * 128],
                                                                  rhs=wt[:, kc, 0:ncols], start=(kc == 0), stop=(kc == 7)),
                     reads=[(hkey, kc), wkey], writes=[psk(bk)], inc=(kc == 7))
            evac(t, pb[:, 0:ncols], psk(bk))

    def gate_prep(t0, nt):
        sl = slice(t0, t0 + nt)
        ig, fl, C, T, U, G, W, tmp, tmp2 = (gsm[k] for k in ("ig", "fl", "C", "T", "u", "g", "w", "tmp", "tmp2"))
        gk = ("gsm", t0)
        g4 = gates[:, sl, :].rearrange("p t (k h) -> p t k h", k=4)
        ig4 = ig[:, sl, :].rearrange("p t (k h) -> p t k h", k=2)
        fl4 = fl[:, sl, :].rearrange("p t (k h) -> p t k h", k=2)
        tm4 = tmp[:, sl, :].rearrange("p t (k h) -> p t k h", k=2)
        for k in range(2):
            P.op("dve", lambda e, k=k: e.tensor_copy(out=ig4[:, :, k, :], in_=g4[:, :, 2 * k, :]),
                 reads=[("gates", t0)], writes=[gk])
            P.op("dve", lambda e, k=k: e.scalar_tensor_tensor(out=tm4[:, :, k, :], in0=g4[:, :, 2 * k + 1, :], scalar=-1.0,
                                                              in1=g4[:, :, 2 * k + 1, :], op0=ALU.mult, op1=ALU.max),
                 reads=[("gates", t0)], writes=[gk])
        P.op("act", lambda e: e.activation(out=tmp[:, sl, :], in_=tmp[:, sl, :], func=AF.Exp, scale=-1.0),
             reads=[gk], writes=[gk])
        P.op("act", lambda e: e.activation(out=tmp[:, sl, :], in_=tmp[:, sl, :], func=AF.Ln, bias=1.0),
             reads=[gk], writes=[gk])
        for k in range(2):
            P.op("dve", lambda e, k=k: e.scalar_tensor_tensor(out=fl4[:, :, k, :], in0=g4[:, :, 2 * k + 1, :], scalar=0.0,
                                                              in1=tm4[:, :, k, :], op0=ALU.min, op1=ALU.subtract),
                 reads=[("gates", t0), gk], writes=[gk])
        pC, pT_, pD0, pD1 = PSB[4], PSB[5], PSB[6], PSB[7]
        for j in range(nt):
            t = t0 + j
            for (mat, mk, pp, bk) in ((triincl, "triincl", pC, 4), (blockones, "blockones", pT_, 5)):
                P.op("pe", lambda e, mat=mat, pp=pp, j=j, t=t: e.matmul(pp[:, j * 8:(j + 1) * 8], lhsT=mat[:],
                                                                         rhs=fl[:, t, :], start=True, stop=True),
                     reads=[mk, gk], writes=[psk(bk)], inc=(j == nt - 1))
        n8 = nt * 8
        P.op("dve", lambda e: e.tensor_copy(out=C[:, sl, :].rearrange("p t k -> p (t k)"), in_=pC[:, 0:n8]),
             reads=[psk(4)], writes=[gk])
        P.op("dve", lambda e: e.tensor_copy(out=T[:, sl, :].rearrange("p t k -> p (t k)"), in_=pT_[:, 0:n8]),
             reads=[psk(5)], writes=[gk])
        P.op("act", lambda e: e.activation(out=dec[:, 0, sl, :], in_=T[:, sl, :], func=AF.Exp), reads=[gk], writes=[gk])
        f = slice(0, 4)
        bsl = slice(4, 8)

        def tt(out, a, b_, op):
            P.op("dve", lambda e: e.tensor_tensor(out=out, in0=a, in1=b_, op=op), reads=[gk], writes=[gk])

        tt(U[:, sl, f], ig[:, sl, f], C[:, sl, f], ALU.subtract)
        tt(G[:, sl, f], C[:, sl, f], C[:, sl, f], ALU.max)
        tt(W[:, sl, f], T[:, sl, f], U[:, sl, f], ALU.add)
        tt(tmp2[:, sl, bsl], T[:, sl, bsl], C[:, sl, bsl], ALU.subtract)
        tt(G[:, sl, bsl], tmp2[:, sl, bsl], fl[:, sl, bsl], ALU.add)
        tt(U[:, sl, bsl], ig[:, sl, bsl], G[:, sl, bsl], ALU.subtract)
        tt(tmp2[:, sl, bsl], C[:, sl, bsl], fl[:, sl, bsl], ALU.subtract)
        tt(W[:, sl, bsl], tmp2[:, sl, bsl], ig[:, sl, bsl], ALU.add)
        for X in (U, G, W):
            P.op("act", lambda e, X=X: e.activation(out=X[:, sl, :], in_=X[:, sl, :], func=AF.Exp), reads=[gk], writes=[gk])
        return gk

    for b in range(NB):
        P.barrier()
        with ExitStack() as sA:
            h1T = sb("h1T", [128, 8, L], BF16, sA)
            with ExitStack() as s1:
                norm_to_T(lambda t: x_d[b, t * 128:(t + 1) * 128, :], NT, b, h1T, "hT", s1, "a")
                if b == 0:
                    dbg_out("h1T", h1T[:], [("hT", kc) for kc in range(8)])
                wb = sb("wb_u", [128, 8, 512], BF16, s1)
                u_sb = sb("u_sb", [128, NT, 512], BF16, s1)
                load_wcols(wb, OFF_F, 512, "wb_u")

                def ev_u(t, pap, pk):
                    if t % 2 == 0:
                        P.op("act", lambda e: e.activation(out=u_sb[:, t, :], in_=pap, func=AF.Copy), reads=[pk], writes=[("u_sb", t)])
                    else:
                        P.op("dve", lambda e: e.tensor_copy(out=u_sb[:, t, :], in_=pap), reads=[pk], writes=[("u_sb", t)])
                proj_tok(h1T, NT, wb, "wb_u", 512, ev_u, [2, 3])
                dft = [sb(f"dft{i}", [128, 8, 512], BF16, s1) for i in range(3)]
                ueo = [sb(f"ueo{i}", [128, 8, 512], BF16, s1) for i in range(2)]
                pq2 = [sb(f"pq2{i}", [128, 1024], BF16, s1) for i in range(4)]
                for lc in range(8):
                    P.op("dve", lambda e, lc=lc: e.tensor_tensor(out=ueo[0][:, lc, :], in0=u_sb[:, lc, :], in1=u_sb[:, lc + 8, :], op=ALU.add),
                         reads=[("u_sb", lc), ("u_sb", lc + 8)], writes=[("ueo", 0, lc)])
                    P.op("pool", lambda e, lc=lc: e.tensor_tensor(out=ueo[1][:, lc, :], in0=u_sb[:, lc, :], in1=u_sb[:, lc + 8, :], op=ALU.subtract),
                         reads=[("u_sb", lc), ("u_sb", lc + 8)], writes=[("ueo", 1, lc)])
                it = 0
                dft_seq = [(j, m, nm, e_) for j in range(2) for m, nm in enumerate(("dftC", "dftS")) for e_ in range(2)]

                def dft_load(k):
                    if k < len(dft_seq):
                        j_, m_, nm_, ee_ = dft_seq[k]
                        P.dma("sp", dft[k % 3][:], cd[nm_][ee_ * 2 + j_], writes=[("dft", k % 3)])
                for k in range(3):
                    dft_load(k)
                for j in range(2):
                    for m, nm in enumerate(("dftC", "dftS")):
                        for e_ in range(2):
                            dt_ = dft[it % 3]
                            for cc in range(4):
                                bk = (it % 2) * 4 + cc
                                pb = PSB[bk]
                                for lc in range(8):
                                    P.op("pe", lambda e, cc=cc, lc=lc, pb=pb, dt_=dt_, e_=e_: e.matmul(
                                        pb[:, :], lhsT=ueo[e_][:, lc, cc * 128:(cc + 1) * 128], rhs=dt_[:, lc, :],
                                        start=(lc == 0), stop=(lc == 7)),
                                         reads=[("ueo", e_, lc), ("dft", it % 3)], writes=[psk(bk)], inc=(lc == 7))
                                q = pq2[cc][:].rearrange("p (n two) -> p n two", two=2)[:, :, e_]
                                if cc % 2 == 0:
                                    P.op("act", lambda e, q=q, pb=pb: e.activation(out=q, in_=pb[:, :], func=AF.Copy),
                                         reads=[psk(bk)], writes=[("pqs", cc)])
                                else:
                                    P.op("dve", lambda e, q=q, pb=pb: e.tensor_copy(out=q, in_=pb[:, :]),
                                         reads=[psk(bk)], writes=[("pqs", cc)])
                                if e_ == 1:
                                    P.dma("pool", pq_d[m * 4 + cc, :, j * 1024:(j + 1) * 1024], pq2[cc][:], reads=[("pqs", cc)],
                                          writes=["pq_d"])
                            dft_load(it + 3)
                            it += 1
                P.barrier()
            if b == 0:
                dbg_out("pq", pq_d, ["pq_d"])
            with ExitStack() as s2:
                gates = sb("gates", [128, NT + 2, 16], F32, s2)
                gsm = {nm: sb("g_" + nm, [128, NT + 2, 8], F32, s2) for nm in ("ig", "fl", "C", "T", "u", "g", "w", "tmp", "tmp2")}
                dec = sb("dec", [128, 2, NT + 2, 8], F32, s2)
                hcT = sb("hcT", [128, 8, CL], BF16, s2)
                qkT = sb("qkT", [128, 8, L], BF16, s2)
                kcT = sb("kcT", [128, 4, CL], BF16, s2)
                vex = sb("vex", [128, NT + 2, 4, 130], BF16, s2)
                s2a = ExitStack()
                norm_to_T(lambda t: ctx_d[b, t * 128:(t + 1) * 128, :], 2, 4, hcT, "hcT", s2a, "c")
                diag = sb("diag", [128, 72, 128], BF16, s2a)
                for kc in range(8):
                    for tap in range(9):
                        if (kc * 9 + tap) % 2 == 0:
                            P.op("dve", lambda e, kc=kc, tap=tap: e.tensor_scalar(out=diag[:, kc * 9 + tap, :], in0=ident32[:],
                                                                                  scalar1=wcT[:, kc, tap:tap + 1], scalar2=None,
                                                                                  op0=ALU.mult),
                                 reads=["ident32", "wcT"], writes=[("diag", kc)])
                        else:
                            P.op("act", lambda e, kc=kc, tap=tap: e.activation(out=diag[:, kc * 9 + tap, :], in_=ident32[:], func=AF.Copy,
                                                                               scale=wcT[:, kc, tap:tap + 1]),
                                 reads=["ident32", "wcT"], writes=[("diag", kc)])
                wbs = [sb(f"wbs{i}", [128, 8, 512], BF16, s2a) for i in range(2)]
                cin = [sb(f"cin{i}", [128, 66 + L + 66], BF16, s2a) for i in range(3)]
                for i in range(3):
                    P.op("dve", lambda e, i=i: e.memset(cin[i][:], 0.0), writes=[("cin", i)])
                P.op("dve", lambda e: e.memset(vex[:], 1.0), writes=["vex"])
                load_wcols(wbs[0], OFF_V, 512, ("wbs", 0))
                load_wcols(wbs[1], OFF_G, 16, ("wbs", 1), q="act")

                def ev_v(off):
                    def f(t, pap, pk):
                        o = vex[:, off + t, :, 0:128]
                        i_ = pap.rearrange("p (h d) -> p h d", h=4)
                        if t % 2 == 0:
                            P.op("act", lambda e: e.activation(out=o, in_=i_, func=AF.Copy), reads=[pk], writes=["vex"])
                        else:
                            P.op("dve", lambda e: e.tensor_copy(out=o, in_=i_), reads=[pk], writes=["vex"])
                    return f

                def ev_g(off):
                    def f(t, pap, pk):
                        P.op("dve", lambda e: e.tensor_tensor(out=gates[:, off + t, :], in0=pap, in1=bg_sb[:], op=ALU.add),
                             reads=[pk, "bg"], writes=[("gates", off)])
                    return f
                proj_tok(h1T, NT, wbs[0], ("wbs", 0), 512, ev_v(0), [0, 1])
                proj_tok(hcT, 2, wbs[0], ("wbs", 0), 512, ev_v(NT), [0, 1], hkey="hcT")
                proj_tok(h1T, NT, wbs[1], ("wbs", 1), 16, ev_g(0), [2, 3])
                proj_tok(hcT, 2, wbs[1], ("wbs", 1), 16, ev_g(NT), [2, 3], hkey="hcT")
                if b == 0:
                    dbg_out("gates", gates[:], [("gates", 0), ("gates", NT)])
                for grp in range(2):
                    w_ = wbs[grp % 2]
                    load_wcols(w_, OFF_QK + grp * 512, 512, ("wbs", grp % 2), q="sp")
                    for c4 in range(4):
                        cc = grp * 4 + c4
                        srcs = [("lat", h1T, L)] + ([("ctx", hcT, CL)] if grp == 1 else [])
                        for (kind, hT_, ntok) in srcs:
                            nblk = max(1, ntok // 512)
                            nn = min(512, ntok)
                            for nb_ in range(nblk):
                                pb = PSB[nb_]
                                for kc in range(8):
                                    P.op("pe", lambda e, kc=kc, nb_=nb_, pb=pb, hT_=hT_, w_=w_, c4=c4, nn=nn: e.matmul(
                                        pb[:, 0:nn], lhsT=w_[:, kc, c4 * 128:(c4 + 1) * 128],
                                        rhs=hT_[:, kc, nb_ * 512:nb_ * 512 + nn], start=(kc == 0), stop=(kc == 7)),
                                         reads=[("hT", kc), ("hcT", kc), ("wbs", grp % 2)], writes=[psk(nb_)], inc=(kc == 7))
                                P.op("act", lambda e, nb_=nb_, pb=pb, nn=nn: e.activation(
                                    out=cin[0][:, 66 + nb_ * 512:66 + nb_ * 512 + nn], in_=pb[:, 0:nn], func=AF.Copy),
                                     reads=[psk(nb_)], writes=[("cin", 0)])
                                if kind == "lat":
                                    for i_ in (1, 2):
                                        P.op("act", lambda e, nb_=nb_, pb=pb, i_=i_: e.activation(
                                            out=cin[i_][:, 66 + nb_ * 512:66 + nb_ * 512 + 512], in_=pb[:, 0:512], func=AF.Copy),
                                             reads=[psk(nb_)], writes=[("cin", i_)])
                            if kind == "lat":
                                for i_, xz in ((1, 63), (2, 0)):
                                    P.op("dve", lambda e, i_=i_, xz=xz: e.memset(
                                        cin[i_][:, 66:66 + L].rearrange("p (r x) -> p r x", x=64)[:, :, xz:xz + 1], 0.0),
                                         writes=[("cin", i_)])
                            if kind == "lat":
                                taps = [(dy, dx) for dy in range(3) for dx in range(3)]
                            else:
                                P.op("pool", lambda e: e.memset(cin[0][:, 66 + CL:66 + CL + 2], 0.0), writes=[("cin", 0)])
                                taps = [(1, dx) for dx in range(3)]
                            for nb_ in range(nblk):
                                pb = PSB[4 + nb_]
                                for ti, (dy, dx) in enumerate(taps):
                                    src = cin[{0: 1, 1: 0, 2: 2}[dx]] if kind == "lat" else cin[0]
                                    sh = 64 * (dy - 1) + (dx - 1)
                                    o0 = 66 + nb_ * 512 + sh
                                    P.op("pe", lambda e, pb=pb, src=src, o0=o0, nn=nn, dy=dy, dx=dx, ti=ti, cc=cc, taps=taps: e.matmul(
                                        pb[:, 0:nn], lhsT=diag[:, cc * 9 + dy * 3 + dx, :], rhs=src[:, o0:o0 + nn],
                                        start=(ti == 0), stop=(ti == len(taps) - 1)),
                                         reads=[("diag", cc), ("cin", 0), ("cin", 1), ("cin", 2)], writes=[psk(4 + nb_)],
                                         inc=(ti == len(taps) - 1))
                                dst = qkT[:, cc, nb_ * 512:nb_ * 512 + nn] if kind == "lat" else kcT[:, c4, 0:nn]
                                P.op("act", lambda e, pb=pb, dst=dst, nn=nn: e.activation(out=dst, in_=pb[:, 0:nn], func=AF.Silu),
                                     reads=[psk(4 + nb_)], writes=["qkT" if kind == "lat" else "kcT"])
                            if kind == "ctx":
                                pass
                if b == 0:
                    dbg_out("qkT", qkT[:], ["qkT"])
                    dbg_out("kcT", kcT[:], ["kcT"])
                P.barrier()
                s2a.close()
                gk_l = gate_prep(0, NT)
                gk_c = gate_prep(NT, 2)
                if b == 0:
                    for nm in ("u", "g", "w"):
                        dbg_out("gs_" + nm, gsm[nm][:], [gk_l, gk_c])
                    dbg_out("dec", dec[:], [gk_l, gk_c])
                P.op("dve", lambda e: e.memset(S32[:], 0.0), writes=[("S", i) for i in range(8)])
                P.op("pool", lambda e: e.memset(Sbf[:], 0.0), writes=[("Sbf", i) for i in range(8)])
                NR = 4
                kt = [sb(f"kt{i}", [128, 128], BF16, s2) for i in range(NR)]
                vt = [sb(f"vt{i}", [128, 130], BF16, s2) for i in range(NR)]
                vw = [sb(f"vw{i}", [128, 130], BF16, s2) for i in range(NR)]
                smt = [sb(f"smt{i}", [128, 128], BF16, s2) for i in range(NR)]
                hsm = [sb(f"hsm{i}", [128, 4], F32, s2) for i in range(NR)]
                hbuf = sb("hbuf", [128, NT, 512], BF16, s2)
                hsum = [sb(f"hsum{i}", [128, 512], F32, s2) for i in range(2)]
                ogs = [sb(f"ogs{i}", [128, 512], F32, s2) for i in range(2)]
                hmt = [sb(f"hmt{i}", [128, 512], BF16, s2) for i in range(2)]
                wog = sb("wog", [128, 8, 512], BF16, s2)
                load_wcols(wog, OFF_O, 512, "wog", q="act")
                rr = [0, 0]

                units = []

                def add_tile(di, tg, kTsrc, qTsrc, tcol, outputs, lt, first):
                    for h in range(4):
                        units.append(dict(di=di, tg=tg, kTsrc=kTsrc, qTsrc=qTsrc, tcol=tcol, outputs=outputs, lt=lt, first=first, h=h))

                def stage1(u, k):
                    di, tg, h, tcol, kTsrc, qTsrc = u["di"], u["tg"], u["h"], u["tcol"], u["kTsrc"], u["qTsrc"]
                    if h == 0:
                        prepass_step()
                        u["fi"] = rr[1] % 2
                        if u["outputs"] and not u["first"]:
                            rr[1] += 1
                    else:
                        u["fi"] = units[k - 1]["fi"]
                    r_ = k % NR
                    u["r"] = r_
                    gi = di * 4 + h
                    pk_ = PSB[0][:].bitcast(BF16)[:, r_ * 128:(r_ + 1) * 128]
                    P.op("pe", lambda e: e.transpose(out=pk_, in_=kTsrc[:, h, tcol:tcol + 128], identity=identbf[:]),
                         reads=["qkT", "kcT", "identbf"], writes=[("ps0", r_)])
                    P.op("act", lambda e: e.activation(out=kt[r_][:], in_=pk_, func=AF.Copy, scale=DH ** -0.5),
                         reads=[("ps0", r_)], writes=[("kt", r_)])
                    P.op("act", lambda e: e.activation(out=vw[r_][:, 0:130], in_=vex[:, tg, h, 0:130], func=AF.Copy,
                                                       scale=gsm["w"][:, tg, gi:gi + 1]),
                         reads=["vex", ("gsm", 0), ("gsm", NT)], writes=[("vw", r_)])
                    if u["outputs"]:
                        P.op("act", lambda e: e.activation(out=vt[r_][:, 0:130], in_=vex[:, tg, h, 0:130], func=AF.Copy,
                                                           scale=gsm["u"][:, tg, gi:gi + 1]),
                             reads=["vex", ("gsm", 0), ("gsm", NT)], writes=[("vt", r_)])
                        pS = PSB[1][:, r_ * 128:(r_ + 1) * 128]
                        P.op("pe", lambda e: e.matmul(pS, lhsT=kTsrc[:, h, tcol:tcol + 128], rhs=qTsrc[:, h, tcol:tcol + 128],
                                                      start=True, stop=True),
                             reads=["qkT"], writes=[("ps1", r_)])
                        mk = maskF if di == 0 else maskB
                        P.op("dve", lambda e: e.tensor_tensor(out=smt[r_][:], in0=pS, in1=mk[:], op=ALU.mult),
                             reads=[("ps1", r_), "maskF", "maskB"], writes=[("smt", r_)])

                def stage2(u, k):
                    di, tg, h, tcol, qTsrc, r_ = u["di"], u["tg"], u["h"], u["tcol"], u["qTsrc"], u["r"]
                    sidx = di * 4 + h
                    gi = di * 4 + h
                    if u["outputs"]:
                        pO = PSB[2 + (r_ % 2)][:, (r_ // 2) * 256:(r_ // 2) * 256 + 129]
                        kO = ("pO", r_)
                        u["pO"], u["kO"] = pO, kO
                        P.op("pe", lambda e: e.matmul(pO, lhsT=smt[r_][:], rhs=vt[r_][:, 0:129], start=True, stop=False, skip_group_check=True),
                             reads=[("smt", r_), ("vt", r_)], writes=[kO], inc=False)
                        P.op("pe", lambda e: e.matmul(pO, lhsT=qTsrc[:, h, tcol:tcol + 128], rhs=Sbf[:, sidx, 0:129],
                                                      start=False, stop=True, skip_group_check=True),
                             reads=["qkT", ("Sbf", sidx)], writes=[kO])
                    bu = 4 + (k % 2)
                    pU = PSB[bu][:, 0:129]
                    kU = psk(bu)
                    P.op("pe", lambda e: e.matmul(pU, lhsT=kt[r_][:, :], rhs=vw[r_][:, 0:129], start=True, stop=True),
                         reads=[("kt", r_), ("vw", r_)], writes=[kU])
                    P.op("dve", lambda e: e.scalar_tensor_tensor(
                        out=S32[:, sidx, 0:129], in0=S32[:, sidx, 0:129], scalar=dec[:, 0, tg, gi:gi + 1], in1=pU,
                        op0=ALU.mult, op1=ALU.add),
                         reads=[kU, ("S", sidx), ("gsm", 0), ("gsm", NT)], writes=[("S", sidx)])
                    P.op("act", lambda e: e.activation(out=Sbf[:, sidx, 0:130], in_=S32[:, sidx, 0:130], func=AF.Copy),
                         reads=[("S", sidx)], writes=[("Sbf", sidx)])

                def stage3(u, k):
                    if not u["outputs"]:
                        return
                    di, tg, h, r_, lt, fi = u["di"], u["tg"], u["h"], u["r"], u["lt"], u["fi"]
                    gi = di * 4 + h
                    pO, kO = u["pO"], u["kO"]
                    hs = hsm[r_]
                    gcol = gsm["g"][:, tg, gi:gi + 1]
                    P.op("dve", lambda e: e.tensor_scalar(out=hs[:, 3:4], in0=pO[:, 128:129], scalar1=gcol, scalar2=None, op0=ALU.mult),
                         reads=[kO, ("gsm", 0)], writes=[("hsm", r_)])
                    P.op("dve", lambda e: e.scalar_tensor_tensor(out=hs[:, 0:1], in0=hs[:, 3:4], scalar=-1.0, in1=hs[:, 3:4],
                                                                 op0=ALU.mult, op1=ALU.max),
                         reads=[("hsm", r_)], writes=[("hsm", r_)])
                    P.op("dve", lambda e: e.tensor_scalar(out=hs[:, 0:1], in0=hs[:, 0:1], scalar1=1.0, scalar2=None, op0=ALU.max),
                         reads=[("hsm", r_)], writes=[("hsm", r_)])
                    P.op("dve", lambda e: e.reciprocal(out=hs[:, 1:2], in_=hs[:, 0:1]), reads=[("hsm", r_)], writes=[("hsm", r_)])
                    P.op("dve", lambda e: e.tensor_tensor(out=hs[:, 2:3], in0=hs[:, 1:2], in1=gcol, op=ALU.mult),
                         reads=[("hsm", r_), ("gsm", 0)], writes=[("hsm", r_)])
                    if u["first"]:
                        P.op("dve", lambda e: e.tensor_scalar(out=hbuf[:, lt, h * 128:(h + 1) * 128], in0=pO[:, 0:128],
                                                              scalar1=hs[:, 2:3], scalar2=None, op0=ALU.mult),
                             reads=[kO, ("hsm", r_)], writes=[("hbuf", lt)])
                    else:
                        P.op("dve", lambda e: e.scalar_tensor_tensor(
                            out=hsum[fi][:, h * 128:(h + 1) * 128], in0=pO[:, 0:128], scalar=hs[:, 2:3],
                            in1=hbuf[:, lt, h * 128:(h + 1) * 128], op0=ALU.mult, op1=ALU.add),
                             reads=[kO, ("hsm", r_), ("hbuf", lt)], writes=[("hsum", fi)])
                    if h == 3 and not u["first"]:
                        t = lt
                        pb = PSB[6 + fi]
                        for kc in range(8):
                            P.op("pe", lambda e, kc=kc: e.matmul(pb[:, :], lhsT=h1T[:, kc, t * 128:(t + 1) * 128], rhs=wog[:, kc, :],
                                                                  start=(kc == 0), stop=(kc == 7)),
                                 reads=[("hT", kc), "wog"], writes=[psk(6 + fi)], inc=(kc == 7))
                        P.op("act", lambda e: e.activation(out=ogs[fi][:], in_=pb[:, :], func=AF.Sigmoid),
                             reads=[psk(6 + fi)], writes=[("ogs", fi)])
                        P.op("pool", lambda e: e.tensor_tensor(out=hmt[fi][:], in0=hsum[fi][:], in1=ogs[fi][:], op=ALU.mult),
                             reads=[("hsum", fi), ("ogs", fi)], writes=[("hmt", fi)])
                        pT = PSB[6 + fi][:].bitcast(BF16)
                        for h2 in range(4):
                            P.op("pe", lambda e, h2=h2: e.transpose(out=pT[:, h2 * 128:(h2 + 1) * 128], in_=hmt[fi][:, h2 * 128:(h2 + 1) * 128],
                                                                    identity=identbf[:]),
                                 reads=[("hmt", fi), "identbf"], writes=[psk(6 + fi)], inc=(h2 == 3))
                        P.op("act", lambda e: e.activation(out=hmT[:, :, t * 128:(t + 1) * 128],
                                                           in_=pT[:, 0:512].rearrange("p (h n) -> p h n", h=4), func=AF.Copy),
                             reads=[psk(6 + fi)], writes=["hmT"])

                for i in range(2):
                    add_tile(0, NT + i, kcT, kcT, i * 128, False, i, True)
                    add_tile(1, NT + 1 - i, kcT, kcT, (1 - i) * 128, False, 1 - i, True)
                for s_ in range(NT):
                    tf, tb = s_, NT - 1 - s_
                    first = s_ < NT // 2
                    add_tile(0, tf, qkT[:, 4:8, :], qkT[:, 0:4, :], tf * 128, True, tf, first)
                    add_tile(1, tb, qkT[:, 4:8, :], qkT[:, 0:4, :], tb * 128, True, tb, first)
                stage1(units[0], 0)
                stage1(units[1], 1)
                for k, u in enumerate(units):
                    stage2(u, k)
                    if k + 2 < len(units):
                        stage1(units[k + 2], k + 2)
                    stage3(u, k)
                    if b == 0 and k == 15:
                        dbg_out("S_b", S32[:], [("S", i) for i in range(8)], dst=dbg_d.get("S_b"))
                if b == 0:
                    dbg_out("S_f", S32[:], [("S", i) for i in range(8)])
                if b == 0:
                    dbg_out("hmT", hmT[:], ["hmT"])
                P.barrier()
            with ExitStack() as s3:
                wfold = sb("wfold", [128, 8, D], BF16, s3)
                wm = sb("wm", [128, 4, D], BF16, s3)
                wout = sb("wout", [128, 8, D], BF16, s3)
                wbr = sb("wbr", [128, 8, 2048], BF16, s3)
                ga1 = sb("ga1", [128, D], F32, s3)
                pqb = sb("pqb", [128, 8, 512], BF16, s3)
                mergedT = sb("mergedT", [128, 8, 512], BF16, s3)
                sgF2 = [sb(f"sgF{i}", [128, 512], F32, s3) for i in range(2)]
                sgM2 = [sb(f"sgM{i}", [128, 512], F32, s3) for i in range(2)]
                m12 = [sb(f"m1{i}", [128, 512], F32, s3) for i in range(2)]
                xt2 = [sb("xt2_0", [128, D], F32, s3)] * 2
                x1t = [sb(f"x1t{i}", [128, D], F32, s3) for i in range(2)]
                x1n = sb("x1n", [128, D], F32, s3)
                x1nb = sb("x1nb", [128, D], BF16, s3)
                junk3 = sb("junk3", [128, D], BF16, s3)
                hx2T = sb("hx2T", [128, 8, 128], F32, s3)
                sm3 = sb("sm3", [128, 8], F32, s3)
                sm3x = [sb(f"sm3x{i}", [128, 4], F32, s3) for i in range(2)]
                esb = sb("esb", [128, 16], F32, s3)
                aff = sb("aff", [128, 16], F32, s3)
                afft = [sb(f"afft{i}", [16, 128], F32, s3) for i in range(2)]
                P.dma("pool", pqb[:], pq_d[:, :, 0:512].rearrange("k p n -> p k n"), reads=["pq_d"], writes=["pqb"])
                for hf_ in range(2):
                    cs_ = slice(hf_ * 512, (hf_ + 1) * 512)
                    P.dma("sp", wfold[:, :, cs_], wfold_d[:, cs_].rearrange("(kc p) n -> p kc n", p=128), reads=["wfold_d"],
                          writes=[("wfold", hf_)])
                    P.dma("act", wm[:, :, cs_], wmbf_d[:, cs_].rearrange("(kc p) n -> p kc n", p=128), reads=["wmbf_d"],
                          writes=[("wm", hf_)])
                    for g_ in range(2 * hf_, 2 * hf_ + 2):
                        for fm in range(2):
                            c0_ = fm * 1024 + g_ * 256
                            P.dma("act" if fm == 0 else "sp", wbr[:, :, c0_:c0_ + 256],
                                  winbf_d[:, OFF_BR + c0_:OFF_BR + c0_ + 256].rearrange("(kc p) n -> p kc n", p=128),
                                  reads=["winbf_d"], writes=[("wbr", g_)])
                P.dma("act", wout[:], woutbf_d.rearrange("(kc p) n -> p kc n", p=128), reads=["woutbf_d"], writes=["wout"])
                P.dma("sp", ga1[:], mod_d[b:b + 1, 2 * D:3 * D].partition_broadcast(128), reads=["mod_d"], writes=["ga1"])
                for kc in range(8):
                    if kc % 2 == 0:
                        P.op("dve", lambda e, kc=kc: e.tensor_tensor(out=wout[:, kc, :], in0=wout[:, kc, :], in1=ga1[:], op=ALU.mult),
                             reads=["wout", "ga1"], writes=["wout"])
                    else:
                        P.op("pool", lambda e, kc=kc: e.tensor_tensor(out=wout[:, kc, :], in0=wout[:, kc, :], in1=ga1[:], op=ALU.mult),
                             reads=["wout", "ga1"], writes=["wout"])
                for nb_ in range(4):
                    tsl = slice(nb_ * 512, (nb_ + 1) * 512)
                    for dc in range(8):
                        dsl = slice(dc * 128, (dc + 1) * 128)
                        p4 = (dc % 2) * 4
                        sF, sM, m1_ = sgF2[dc % 2], sgM2[dc % 2], m12[dc % 2]
                        kF, kM, k1 = ("sgF", dc % 2), ("sgM", dc % 2), ("m1", dc % 2)
                        for kc in range(8):
                            P.op("pe", lambda e, kc=kc, dsl=dsl, p4=p4: e.matmul(PSB[p4][:, :], lhsT=wfold[:, kc, dsl], rhs=pqb[:, kc, :],
                                                                                  start=(kc == 0), stop=(kc == 7)),
                                 reads=[("wfold", dc // 4), "pqb"], writes=[psk(p4)], inc=(kc == 7))
                        for kc in range(4):
                            P.op("pe", lambda e, kc=kc, dsl=dsl, tsl=tsl, p4=p4: e.matmul(PSB[p4 + 1][:, :], lhsT=wm[:, kc, dsl], rhs=hmT[:, kc, tsl],
                                                                                           start=(kc == 0), stop=(kc == 3)),
                                 reads=[("wm", dc // 4), "hmT"], writes=[psk(p4 + 1)], inc=(kc == 3))
                        for gi_, bk in ((0, p4 + 2), (1, p4 + 3)):
                            for kc in range(8):
                                P.op("pe", lambda e, kc=kc, dc=dc, tsl=tsl, gi_=gi_, bk=bk: e.matmul(
                                    PSB[bk][:, :], lhsT=wbr[:, kc, gi_ * 1024 + dc * 128:gi_ * 1024 + (dc + 1) * 128],
                                    rhs=h1T[:, kc, tsl], start=(kc == 0), stop=(kc == 7)),
                                     reads=[("wbr", dc // 2), ("hT", kc)], writes=[psk(bk)], inc=(kc == 7))
                        P.op("act", lambda e, sF=sF, p4=p4: e.activation(out=sF[:], in_=PSB[p4 + 2][:, :], func=AF.Sigmoid), reads=[psk(p4 + 2)], writes=[kF])
                        P.op("act", lambda e, sM=sM, p4=p4: e.activation(out=sM[:], in_=PSB[p4 + 3][:, :], func=AF.Sigmoid), reads=[psk(p4 + 3)], writes=[kM])
                        P.op("dve", lambda e, sF=sF, m1_=m1_, p4=p4: e.tensor_tensor(out=m1_[:], in0=PSB[p4][:, :], in1=sF[:], op=ALU.mult),
                             reads=[psk(p4), kF], writes=[k1])
                        P.op("dve", lambda e, sM=sM, p4=p4: e.tensor_tensor(out=sM[:], in0=PSB[p4 + 1][:, :], in1=sM[:], op=ALU.mult),
                             reads=[psk(p4 + 1), kM], writes=[kM])
                        P.op("pool", lambda e, dc=dc, m1_=m1_, sM=sM: e.tensor_tensor(out=mergedT[:, dc, :], in0=m1_[:], in1=sM[:], op=ALU.add),
                             reads=[k1, kM], writes=["mergedT"])
                    if nb_ + 1 < 4:
                        P.dma("sp", pqb[:], pq_d[:, :, (nb_ + 1) * 512:(nb_ + 2) * 512].rearrange("k p n -> p k n"),
                              reads=["pq_d"], writes=["pqb"])

                    def X1(tt):
                        t = nb_ * 4 + tt
                        i = t % 2
                        rows = slice(t * 128, (t + 1) * 128)
                        prepass_step()
                        P.dma("act", xt2[i][:], x_d[b, rows, :], writes=[("xt2", 0)])
                        for half in range(2):
                            hs_ = slice(half * 512, (half + 1) * 512)
                            for kc in range(8):
                                P.op("pe", lambda e, kc=kc, half=half, hs_=hs_: e.matmul(
                                    PSB[4 + half][:, :], lhsT=mergedT[:, kc, tt * 128:(tt + 1) * 128], rhs=wout[:, kc, hs_],
                                    start=(kc == 0), stop=(kc == 7)),
                                     reads=["mergedT", "wout"], writes=[psk(4 + half)], inc=(kc == 7))
                            P.op("dve", lambda e, half=half, hs_=hs_: e.tensor_tensor(out=x1t[i][:, hs_], in0=PSB[4 + half][:, :],
                                                                                      in1=xt2[i][:, hs_], op=ALU.add),
                                 reads=[psk(4 + half), ("xt2", 0)], writes=[("x1t", i)])
                        P.dma("sp", x1_d[b][rows, :], x1t[i][:], reads=[("x1t", i)], writes=[("x1_d", b)])
                        P.op("dve", lambda e: e.scalar_tensor_tensor(out=junk3[:], in0=x1t[i][:], scalar=1.0, in1=x1t[i][:], op0=ALU.mult,
                                                                     op1=ALU.mult, accum_out=sm3x[i][:, 0:1]),
                             reads=[("x1t", i)], writes=[f"ss3{i}junk", f"ss3{i}"])
                        P.op("dve", lambda e: e.tensor_scalar(out=sm3x[i][:, 1:2], in0=sm3x[i][:, 0:1], scalar1=1.0 / D, scalar2=EPS,
                                                              op0=ALU.mult, op1=ALU.add),
                             reads=[f"ss3{i}"], writes=[f"ss3{i}rt"])
                        P.op("pool", lambda e: e.tensor_tensor(out=sm3x[i][:, 2:3], in0=sm3x[i][:, 1:2], in1=mhalf_sb[:, 0:1], op=ALU.pow),
                             reads=[f"ss3{i}rt", "mhalf"], writes=[f"ss3{i}rstd"])

                    def X2(tt):
                        t = nb_ * 4 + tt
                        i = t % 2
                        rows = slice(t * 128, (t + 1) * 128)
                        P.op("dve", lambda e: e.tensor_scalar(out=x1n[:], in0=x1t[i][:], scalar1=sm3x[i][:, 2:3], scalar2=None, op0=ALU.mult),
                             reads=[("x1t", i), f"ss3{i}rstd"], writes=["x1n"])
                        P.op("act", lambda e: e.activation(out=x1nb[:], in_=x1t[i][:], func=AF.Copy, scale=sm3x[i][:, 2:3]),
                             reads=[("x1t", i), f"ss3{i}rstd"], writes=["x1nb"])
                        P.dma("pool", x1n_d[b][rows, :], x1nb[:], reads=["x1nb"], writes=[("x1n_d", b)])

                    def Y(tt):
                        t = nb_ * 4 + tt
                        i = t % 2
                        rows = slice(t * 128, (t + 1) * 128)
                        for kc in range(8):
                            bk = 6 + kc // 4
                            P.op("pe", lambda e, kc=kc, bk=bk: e.transpose(out=PSB[bk][:, (kc % 4) * 128:(kc % 4 + 1) * 128],
                                                                           in_=x1n[:, kc * 128:(kc + 1) * 128], identity=ident32[:]),
                                 reads=["x1n", "ident32"], writes=[psk(bk)], inc=(kc % 4 == 3))
                        for kc in range(8):
                            bk = 6 + kc // 4
                            src = PSB[bk][:, (kc % 4) * 128:(kc % 4 + 1) * 128]
                            if bk == 6:
                                P.op("act", lambda e, kc=kc, src=src: e.activation(out=hx2T[:, kc, :], in_=src, func=AF.Identity,
                                                                                   scale=scale2T[:, kc, b:b + 1], bias=modT[:, 24 + kc, b:b + 1]),
                                     reads=[psk(bk), "scale2T", "modT"], writes=[("hx2T", kc)])
                            else:
                                P.op("dve", lambda e, kc=kc, src=src: e.tensor_scalar(out=hx2T[:, kc, :], in0=src, scalar1=scale2T[:, kc, b:b + 1],
                                                                                      scalar2=modT[:, 24 + kc, b:b + 1], op0=ALU.mult, op1=ALU.add),
                                     reads=[psk(bk), "scale2T", "modT"], writes=[("hx2T", kc)])
                        for kc in range(8):
                            P.op("pe", lambda e, kc=kc: e.matmul(PSB[0][:, 0:NE], lhsT=hx2T[:, kc, :], rhs=wr_sb[:, kc, :],
                                                                  start=(kc == 0), stop=(kc == 7)),
                                 reads=[("hx2T", kc), "wr"], writes=[psk(0)], inc=(kc == 7))
                        P.op("dve", lambda e: e.tensor_reduce(out=sm3[:, 3:4], in_=PSB[0][:, 0:NE], axis=mybir.AxisListType.X, op=ALU.max),
                             reads=[psk(0)], writes=["sm3mx"])
                        P.op("dve", lambda e: e.tensor_scalar(out=sm3[:, 4:5], in0=sm3[:, 3:4], scalar1=-1.0, scalar2=None, op0=ALU.mult),
                             reads=["sm3mx"], writes=["sm3nmx"])
                        P.op("act", lambda e: e.activation(out=esb[:], in_=PSB[0][:, 0:NE], func=AF.Exp, bias=sm3[:, 4:5], accum_out=sm3[:, 5:6]),
                             reads=[psk(0), "sm3nmx"], writes=["esb", "sm3sum"])
                        P.op("dve", lambda e: e.reciprocal(out=sm3[:, 6:7], in_=sm3[:, 5:6]), reads=["sm3sum"], writes=["sm3r"])
                        P.op("dve", lambda e: e.tensor_scalar(out=aff[:], in0=esb[:], scalar1=sm3[:, 6:7], scalar2=None, op0=ALU.mult),
                             reads=["esb", "sm3r"], writes=["aff"])
                        P.op("pe", lambda e: e.transpose(out=PSB[1][0:NE, 0:128], in_=aff[:, :], identity=ident32[:]),
                             reads=["aff", "ident32"], writes=[psk(1)])
                        P.op("dve", lambda e: e.tensor_copy(out=afft[i][:], in_=PSB[1][0:NE, 0:128]), reads=[psk(1)], writes=[("afft", i)])
                        P.dma("sp", affT_all[b * NE:(b + 1) * NE, rows], afft[i][:], reads=[("afft", i)], writes=["affT_all"])

                    X1(0)
                    X2(0)
                    for tt in range(4):
                        if tt + 1 < 4:
                            X1(tt + 1)
                        Y(tt)
                        if tt + 1 < 4:
                            X2(tt + 1)
                if b == 0:
                    dbg_out("x1", x1_d[0], [("x1_d", 0)])
                    dbg_out("affT", affT_all[0:NE, :], ["affT_all"])
                P.barrier()
    bst.close()
    P.barrier()
    NP = NB * NE
    NS = NB * CAP
    if skip_moe:
        es.close()
        return nc
    with ExitStack() as sm:
        vals = sb("vals", [NP, CAP], F32, sm)
        idxu = sb("idxu", [NP, CAP], U32, sm)
        idxf = sb("idxf", [NP, CAP], F32, sm)
        idxT = sb("idxT", [128, 2, 64], I32, sm)
        valsT = sb("valsT", [128, 2, 64], F32, sm)
        ga2 = [sb(f"ga2{i}", [128, D], F32, sm) for i in range(NB)]
        for i in range(NB):
            P.dma("sp", ga2[i][:], mod_d[i:i + 1, 5 * D:6 * D].partition_broadcast(128), reads=["mod_d"], writes=[("ga2", i)])
        A = affT_all[0:NP, :]
        for r in range(CAP // 8):
            rs = slice(r * 8, (r + 1) * 8)
            P.op("dve", lambda e, rs=rs: e.max(out=vals[:, rs], in_=A), reads=["affT_all"], writes=["vals"])
            P.op("dve", lambda e, rs=rs: e.max_index(out=idxu[:, rs], in_max=vals[:, rs], in_values=A),
                 reads=["affT_all", "vals"], writes=["idxu"])
            P.op("dve", lambda e, rs=rs: e.match_replace(out=A, in_to_replace=vals[:, rs], in_values=A, imm_value=-1.0),
                 reads=["vals"], writes=["affT_all"])
        P.op("dve", lambda e: e.tensor_copy(out=idxf[:], in_=idxu[:]), reads=["idxu"], writes=["idxf"])
        for hh in range(2):
            P.op("pe", lambda e, hh=hh: e.transpose(out=PSB[0][:, hh * 64:hh * 64 + NP], in_=idxf[0:NP, hh * 128:(hh + 1) * 128],
                                                     identity=ident32[0:NP, 0:NP]), reads=["idxf", "ident32"], writes=[psk(0)])
            P.op("pe", lambda e, hh=hh: e.transpose(out=PSB[1][:, hh * 64:hh * 64 + NP], in_=vals[0:NP, hh * 128:(hh + 1) * 128],
                                                     identity=ident32[0:NP, 0:NP]), reads=["vals", "ident32"], writes=[psk(1)])
        for hh in range(2):
            P.op("dve", lambda e, hh=hh: e.tensor_copy(out=idxT[:, hh, 0:NP], in_=PSB[0][:, hh * 64:hh * 64 + NP]), reads=[psk(0)], writes=["idxT"])
            P.op("dve", lambda e, hh=hh: e.tensor_copy(out=valsT[:, hh, 0:NP], in_=PSB[1][:, hh * 64:hh * 64 + NP]), reads=[psk(1)], writes=["valsT"])
        dbg_out("idx", idxu[:], ["idxu"])
        dbg_out("vals", vals[:], ["vals"])
        sx = ExitStack()
        wg = [sb(f"wg{i}", [128, 8, D], BF16, sx) for i in range(2)]
        wu = [sb(f"wu{i}", [128, 8, D], BF16, sx) for i in range(2)]
        wd = [sb(f"wd{i}", [128, 8, D], BF16, sx) for i in range(2)]
        XT2 = [sb(f"XT{i}", [128, 8, NS], BF16, sx) for i in range(2)]
        hidT = sb("hidT", [128, 8, NS], BF16, sx)
        xg = [sb(f"xg{i}", [128, D], BF16, sx) for i in range(2)]
        yt = [sb(f"yt{i}", [128, D], F32, sx) for i in range(2)]
        nn = min(512, NS)
        nblk = NS // nn
        sgt = [sb("sgt0", [128, nn], F32, sx)] * 2

        while not prepass_done():
            prepass_step()

        def load_expert(e_):
            i = e_ % 2
            for m, (wt, nm, q_) in enumerate(((wg, "wg", "sp"), (wu, "wu", "act"), (wd, "wd", "sp"))):
                P.dma(q_, wt[i][:], webf_d[m][e_].rearrange("(kc p) n -> p kc n", p=128), reads=[("webf", m, e_)], writes=[(nm, i)])
        cntg = [0]

        def gather_step(e_, st_):
            b, hh = st_ // 2, st_ % 2
            XT = XT2[e_ % 2]
            kx = ("XT", e_ % 2)
            i = cntg[0] % 2
            cntg[0] += 1
            col = b * NE + e_
            P.dma("pool", None, None, reads=[("x1n_d", b), "idxT"], writes=[("xg", i)],
                  fn=lambda g, i=i, b=b, hh=hh, col=col: g.indirect_dma_start(
                      out=xg[i][:, :], out_offset=None, in_=x1n_d[b][:, :],
                      in_offset=bass.IndirectOffsetOnAxis(ap=idxT[:, hh, col:col + 1], axis=0)))
            pT = PSB[i][:].bitcast(BF16)
            for kc in range(8):
                P.op("pe", lambda e, kc=kc, i=i, pT=pT: e.transpose(out=pT[:, kc * 128:(kc + 1) * 128], in_=xg[i][:, kc * 128:(kc + 1) * 128],
                                                                     identity=identbf[:]),
                     reads=[("xg", i), "identbf"], writes=[psk(i)], inc=(kc == 7))
            c0 = st_ * 128
            for kc in range(8):
                src = pT[:, kc * 128:(kc + 1) * 128]
                if i == 0:
                    P.op("act", lambda e, kc=kc, src=src, c0=c0, b=b, XT=XT: e.activation(out=XT[:, kc, c0:c0 + 128], in_=src, func=AF.Identity,
                                                                                          scale=scale2T[:, kc, b:b + 1], bias=modT[:, 24 + kc, b:b + 1]),
                         reads=[psk(i), "scale2T", "modT"], writes=[(kx, kc)])
                else:
                    P.op("dve", lambda e, kc=kc, src=src, c0=c0, b=b, XT=XT: e.tensor_scalar(out=XT[:, kc, c0:c0 + 128], in0=src,
                                                                                             scalar1=scale2T[:, kc, b:b + 1],
                                                                                             scalar2=modT[:, 24 + kc, b:b + 1], op0=ALU.mult, op1=ALU.add),
                         reads=[psk(i), "scale2T", "modT"], writes=[(kx, kc)])

        NST = NB * 2
        for st_ in range(NST):
            gather_step(0, st_)
        load_expert(0)
        for e_ in range(NE):
            if e_ + 1 < NE:
                load_expert(e_ + 1)
            wi = e_ % 2
            XT = XT2[e_ % 2]
            kxt = ("XT", e_ % 2)
            nxt = list(range(NST)) if e_ + 1 < NE else []
            for fc in range(8):
                fsl = slice(fc * 128, (fc + 1) * 128)
                for blk in range(nblk):
                    bsl_ = slice(blk * nn, (blk + 1) * nn)
                    j = (fc * nblk + blk) % 2
                    bg_, bu_ = 2 + j * 2, 3 + j * 2
                    for (wt, nm, bk) in ((wg, "wg", bg_), (wu, "wu", bu_)):
                        for kc in range(8):
                            P.op("pe", lambda e, kc=kc, wt=wt, bk=bk, fsl=fsl, bsl_=bsl_: e.matmul(
                                PSB[bk][:, 0:nn], lhsT=wt[wi][:, kc, fsl], rhs=XT[:, kc, bsl_], start=(kc == 0), stop=(kc == 7)),
                                 reads=[(nm, wi), (kxt, kc)], writes=[psk(bk)], inc=(kc == 7))
                    P.op("act", lambda e, j=j, bg_=bg_: e.activation(out=sgt[j][:], in_=PSB[bg_][:, 0:nn], func=AF.Silu),
                         reads=[psk(bg_)], writes=[("sgt", 0)])
                    P.op("dve", lambda e, j=j, bu_=bu_, fc=fc, bsl_=bsl_: e.tensor_tensor(out=hidT[:, fc, bsl_], in0=PSB[bu_][:, 0:nn], in1=sgt[j][:],
                                                                                          op=ALU.mult),
                         reads=[psk(bu_), ("sgt", 0)], writes=["hidT"])
                per = -(-NST // 8)
                for _ in range(per):
                    if nxt:
                        gather_step(e_ + 1, nxt.pop(0))
            for st_ in range(NB * 2):
                b, hh = st_ // 2, st_ % 2
                col = b * NE + e_
                i = st_ % 2
                for half in range(2):
                    hs_ = slice(half * 512, (half + 1) * 512)
                    bk = (6 + half) if st_ % 2 == 0 else half
                    for fc in range(8):
                        P.op("pe", lambda e, fc=fc, st_=st_, hs_=hs_, bk=bk: e.matmul(PSB[bk][:, :], lhsT=hidT[:, fc, st_ * 128:(st_ + 1) * 128],
                                                                                       rhs=wd[wi][:, fc, hs_], start=(fc == 0), stop=(fc == 7)),
                             reads=["hidT", ("wd", wi)], writes=[psk(bk)], inc=(fc == 7))
                    P.op("dve", lambda e, i=i, hs_=hs_, bk=bk, hh=hh, col=col, b=b: e.scalar_tensor_tensor(
                        out=yt[i][:, hs_], in0=PSB[bk][:, :], scalar=valsT[:, hh, col:col + 1], in1=ga2[b][:, hs_], op0=ALU.mult, op1=ALU.mult),
                         reads=[psk(bk), "valsT", ("ga2", b)], writes=[("yt", i)])
                P.dma("pool", None, None, reads=[("yt", i), "idxT"], writes=[("x1_d", b)],
                      fn=lambda g, i=i, b=b, hh=hh, col=col: g.indirect_dma_start(
                          out=x1_d[b][:, :], out_offset=bass.IndirectOffsetOnAxis(ap=idxT[:, hh, col:col + 1], axis=0),
                          in_=yt[i][:, :], in_offset=None, compute_op=ALU.add))
        P.barrier()
        sx.close()
        gfin = sb("gfin", [128, D], F32, sm)
        P.dma("sp", gfin[:], gvec_d[2:3, :].partition_broadcast(128), writes=["gfin"])
        NF = 4
        xf = [sb(f"xf{i}", [128, D], F32, sm) for i in range(NF)]
        of = [sb(f"of{i}", [128, D], F32, sm) for i in range(NF)]
        junkf = sb("junkf", [128, D], BF16, sm)
        smf = [sb(f"smf{i}", [128, 4], F32, sm) for i in range(NF)]
        cf = 0
        for b in range(NB):
            for t in range(NT):
                i = cf % NF
                cf += 1
                rows = slice(t * 128, (t + 1) * 128)
                P.dma("sp", xf[i][:], x1_d[b][rows, :], reads=[("x1_d", b)], writes=[("xf", i)])
                rms_rstd("dve", xf[i][:], junkf[:], smf[i][:, 0:1], smf[i][:, 1:2], smf[i][:, 2:3], ("xf", i), f"ssf{i}")
                P.op("dve", lambda e, i=i: e.scalar_tensor_tensor(out=of[i][:], in0=xf[i][:], scalar=smf[i][:, 2:3], in1=gfin[:],
                                                                  op0=ALU.mult, op1=ALU.mult),
                     reads=[("xf", i), f"ssf{i}rstd", "gfin"], writes=[("of", i)])
                P.dma("pool", out_d[b, rows, :], of[i][:], reads=[("of", i)], writes=[("out", b, t)])
        P.barrier()
    es.close()
    return nc


_NC_CACHE = {}


def _core_inputs(inp, bs, consts):
    m = dict(consts)
    m["x"] = np.ascontiguousarray(inp["x"][bs], dtype=np.float32)
    m["ctx"] = np.ascontiguousarray(inp["ctx"][bs], dtype=np.float32)
    cv = np.zeros((5, D), np.float32)
    cc = np.asarray(inp["c"][bs], dtype=np.float32)
    cv[:cc.shape[0]] = cc
    cv[4] = np.asarray(inp["c_ctx"], dtype=np.float32)
    m["cvec"] = cv
    m["w_ada"] = np.ascontiguousarray(inp["w_ada"][0])
    m["b_ada"] = np.ascontiguousarray(inp["b_ada"]).reshape(1, 6 * D)
    m["gvec"] = np.ascontiguousarray(np.stack([inp["g_norm1"][0], inp["g_norm2"][0], inp["g_final"]]))
    m["w_in"] = np.ascontiguousarray(inp["w_in"][0])
    m["b_gates"] = np.ascontiguousarray(inp["b_gates"]).reshape(1, 16)
    m["w_conv"] = np.ascontiguousarray(inp["w_conv"][0]).reshape(9, D)
    m["w_fourier"] = np.ascontiguousarray(inp["w_fourier"][0])
    m["w_mlstm"] = np.ascontiguousarray(inp["w_mlstm"][0])
    m["w_out"] = np.ascontiguousarray(inp["w_out"][0])
    m["w_router"] = np.ascontiguousarray(inp["w_router"][0])
    m["w_gate_e"] = np.ascontiguousarray(inp["w_gate_e"][0])
    m["w_up_e"] = np.ascontiguousarray(inp["w_up_e"][0])
    m["w_down_e"] = np.ascontiguousarray(inp["w_down_e"][0])
    return m


def kernel(**inputs):
    inp = {k: np.asarray(v) for k, v in inputs.items()}
    B = inp["x"].shape[0]
    nb = B // NCORES
    if "c" not in _CONST_CACHE:
        _CONST_CACHE["c"] = _consts()
    consts = _CONST_CACHE["c"]
    if nb not in _NC_CACHE:
        _NC_CACHE[nb] = build(nb)
    nc = _NC_CACHE[nb]
    in_maps = [_core_inputs(inp, slice(i * nb, (i + 1) * nb), consts) for i in range(NCORES)]
    res = run_bass_kernel_spmd(nc, in_maps, core_ids=list(range(NCORES)))
    out = np.concatenate([np.asarray(r["out"]) for r in res.results], axis=0)
    return out.astype(np.float32, copy=False)
```
